# Optimizing a Trainium2 kernel written in Bass

```python
import jax
import jax.numpy as jnp
from jax import lax
import numpy as np


D_MODEL = 1024
BATCH = 8
SEQ = 4096
DEPTH = 2

GRID_W = 64
CTX_LEN = 256

N_HEADS = 16
HEAD_DIM = 64
ATT_W = N_HEADS * HEAD_DIM
WIN_R = 8
WIN_C = 16

FOURIER_GROUPS = 4
FOURIER_W = D_MODEL // 2
FOURIER_GROUP_W = FOURIER_W // FOURIER_GROUPS

POOL_WINDOWS = (2, 4, 8, 16)
POOL_W = D_MODEL // 2
POOL_GROUP_W = POOL_W // len(POOL_WINDOWS)
POOL_OUT_GROUP = D_MODEL // len(POOL_WINDOWS)

N_BRANCHES = 3
K_OFF = 0
V_OFF = K_OFF + ATT_W
Q_OFF = V_OFF + ATT_W
F_OFF = Q_OFF + ATT_W
P_OFF = F_OFF + FOURIER_W
G_OFF = P_OFF + POOL_W
IN_W = G_OFF + N_BRANCHES * D_MODEL

N_EXPERTS = 16
EC_CAPACITY_FACTOR = 2
EXPERT_FF = 2816

RMS_EPS = 1e-6

kernel_name = "hybrid_na_fourier_pool_ecmoe_dit"


def rmsnorm(x, g):
    x32 = x.astype(jnp.float32)
    y = x32 * lax.rsqrt(jnp.mean(x32 * x32, axis=-1, keepdims=True) + RMS_EPS)
    return y.astype(x.dtype) * g


def adaln_params(cond, ada_w, ada_b):
    mod = jax.nn.silu(cond) @ ada_w + ada_b
    return jnp.split(mod, 6, axis=-1)


def modulate(x, g, shift, scale):
    return rmsnorm(x, g) * (1 + scale) + shift


def split_heads(t):
    return t.reshape(t.shape[0], t.shape[1], N_HEADS, HEAD_DIM)


def neighbourhood_attention(q, k, v, k_ctx, v_ctx, rpb):
    batch, length, heads, hd = q.shape
    rows = length // GRID_W
    win_r = min(WIN_R, rows)
    win_c = min(WIN_C, GRID_W)
    n_win = win_r * win_c
    qg = q.reshape(batch, rows, GRID_W, heads, hd)
    kg = k.reshape(batch, rows, GRID_W, heads, hd)
    vg = v.reshape(batch, rows, GRID_W, heads, hd)
    col = jnp.arange(GRID_W)
    col_start = jnp.clip(col - win_c // 2, 0, GRID_W - win_c)
    cols_idx = col_start[:, None] + jnp.arange(win_c)[None, :]
    coff = cols_idx - col[:, None] + (WIN_C - 1)
    scale = hd ** -0.5

    def row_block(r):
        rs = jnp.clip(r - win_r // 2, 0, rows - win_r)
        q_r = lax.dynamic_index_in_dim(qg, r, axis=1, keepdims=False)
        k_win = lax.dynamic_slice_in_dim(kg, rs, win_r, axis=1)[:, :, cols_idx]
        v_win = lax.dynamic_slice_in_dim(vg, rs, win_r, axis=1)[:, :, cols_idx]
        roff = rs + jnp.arange(win_r) - r + (WIN_R - 1)
        bias = rpb[:, roff[None, :, None], coff[:, None, :]]
        s_win = jnp.einsum('bqhd,brqjhd->bhqrj', q_r, k_win) * scale + bias[None]
        s_ctx = jnp.einsum('bqhd,bkhd->bhqk', q_r, k_ctx) * scale
        s = jnp.concatenate([s_win.reshape(batch, heads, GRID_W, n_win), s_ctx], axis=-1)
        p = jax.nn.softmax(s.astype(jnp.float32), axis=-1).astype(q.dtype)
        p_win = p[..., :n_win].reshape(batch, heads, GRID_W, win_r, win_c)
        return (jnp.einsum('bhqrj,brqjhd->bqhd', p_win, v_win)
                + jnp.einsum('bhqk,bkhd->bqhd', p[..., n_win:], v_ctx))

    out = lax.map(row_block, jnp.arange(rows))
    return jnp.moveaxis(out, 0, 1).reshape(batch, length, heads * hd)


def context_attention(q, k, v):
    batch, length = q.shape[0], q.shape[1]
    s = jnp.einsum('bqhd,bkhd->bhqk', q, k) * (HEAD_DIM ** -0.5)
    p = jax.nn.softmax(s.astype(jnp.float32), axis=-1).astype(q.dtype)
    return jnp.einsum('bhqk,bkhd->bqhd', p, v).reshape(batch, length, ATT_W)


def fourier_mix(u):
    batch, length, _ = u.shape
    ug = u.reshape(batch, length, FOURIER_GROUPS, FOURIER_GROUP_W).astype(jnp.float32)
    f = jnp.fft.fft2(ug, axes=(1, 3), norm='ortho').real
    return f.reshape(batch, length, FOURIER_W).astype(u.dtype)


def pooling_mix(u, w_pool, pool_scale):
    batch, length, _ = u.shape
    ug = u.reshape(batch, length, len(POOL_WINDOWS), POOL_GROUP_W)
    t = jnp.arange(length)
    outs = []
    for gi, w in enumerate(POOL_WINDOWS):
        ui = ug[:, :, gi].astype(jnp.float32)
        cs = jnp.concatenate([jnp.zeros_like(ui[:, :1]), jnp.cumsum(ui, axis=1)], axis=1)
        lo = jnp.clip(t - w // 2, 0, length)
        hi = jnp.clip(t + w - w // 2, 0, length)
        mean = (cs[:, hi] - cs[:, lo]) / (hi - lo).astype(jnp.float32)[:, None]
        outs.append(mean - ui)
    pooled = jnp.stack(outs, axis=2).astype(u.dtype)
    y = jnp.einsum('blgc,gcf->blgf', pooled, w_pool).reshape(batch, length, D_MODEL)
    return y * pool_scale


def merge_branches(att, p, w_att_o, w_fourier, w_pool, pool_scale, w_out):
    y_att = att @ w_att_o
    y_four = fourier_mix(p[..., F_OFF:P_OFF]) @ w_fourier
    y_pool = pooling_mix(p[..., P_OFF:G_OFF], w_pool, pool_scale)
    g = jax.nn.sigmoid(p[..., G_OFF:IN_W])
    merged = (g[..., :D_MODEL] * y_att
              + g[..., D_MODEL:2 * D_MODEL] * y_four
              + g[..., 2 * D_MODEL:] * y_pool)
    return merged @ w_out


def expert_choice_ffn(h, w_router, w_gate, w_up, w_down):
    batch, length, _ = h.shape
    cap = EC_CAPACITY_FACTOR * length // N_EXPERTS
    affinity = jax.nn.softmax((h @ w_router).astype(jnp.float32), axis=-1)
    gates, idx = lax.top_k(jnp.swapaxes(affinity, 1, 2), cap)
    b_idx = jnp.arange(batch)[:, None, None]
    xg = h[b_idx, idx]
    a = jnp.einsum('becd,edf->becf', xg, w_gate)
    u = jnp.einsum('becd,edf->becf', xg, w_up)
    y = jnp.einsum('becf,efd->becd', jax.nn.silu(a) * u, w_down) * gates[..., None].astype(h.dtype)
    return jnp.zeros_like(h).at[b_idx, idx].add(y)


def trunk_layer(x, ctx, c, c_ctx, ada_w, ada_b, norm1_g, norm2_g, w_in, rpb, w_att_o, w_fourier,
                w_pool, pool_scale, w_out, w_router, w_gate, w_up, w_down, update_ctx):
    sh1, sc1, g1, sh2, sc2, g2 = [m[:, None, :] for m in adaln_params(c, ada_w, ada_b)]
    sh1c, sc1c, g1c, sh2c, sc2c, g2c = adaln_params(c_ctx, ada_w, ada_b)

    h = modulate(x, norm1_g, sh1, sc1)
    hc = modulate(ctx, norm1_g, sh1c, sc1c)
    p = h @ w_in
    pc = hc @ (w_in if update_ctx else w_in[:, :Q_OFF])
    k_ctx = split_heads(pc[..., K_OFF:V_OFF])
    v_ctx = split_heads(pc[..., V_OFF:Q_OFF])
    att = neighbourhood_attention(split_heads(p[..., Q_OFF:F_OFF]), split_heads(p[..., K_OFF:V_OFF]),
                                  split_heads(p[..., V_OFF:Q_OFF]), k_ctx, v_ctx, rpb)
    x = x + g1 * merge_branches(att, p, w_att_o, w_fourier, w_pool, pool_scale, w_out)

    h2 = modulate(x, norm2_g, sh2, sc2)
    x = x + g2 * expert_choice_ffn(h2, w_router, w_gate, w_up, w_down)

    if update_ctx:
        att_c = context_attention(split_heads(pc[..., Q_OFF:F_OFF]), k_ctx, v_ctx)
        ctx = ctx + g1c * merge_branches(att_c, pc, w_att_o, w_fourier, w_pool, pool_scale, w_out)
        hc2 = modulate(ctx, norm2_g, sh2c, sc2c)
        ctx = ctx + g2c * expert_choice_ffn(hc2, w_router, w_gate, w_up, w_down)
    return x, ctx


def _normal(k, shape, scale):
    return jax.random.normal(k, shape, jnp.float32) * scale


def setup_inputs(seed: int = 0) -> dict:
    key = jax.random.key(seed)
    ks = jax.random.split(key, 21)
    d = D_MODEL
    return {
        'x': _normal(ks[0], (BATCH, SEQ, d), 1.0),
        'c': _normal(ks[1], (BATCH, d), 1.0),
        'ctx': _normal(ks[2], (BATCH, CTX_LEN, d), 1.0),
        'c_ctx': _normal(ks[3], (d,), 1.0),
        'ada_w': _normal(ks[4], (DEPTH, d, 6 * d), 0.5 * d ** -0.5),
        'ada_b': _normal(ks[5], (DEPTH, 6 * d), 0.02),
        'norm1_g': 1.0 + _normal(ks[6], (DEPTH, d), 0.02),
        'norm2_g': 1.0 + _normal(ks[7], (DEPTH, d), 0.02),
        'w_in': _normal(ks[8], (DEPTH, d, IN_W), d ** -0.5),
        'rpb': _normal(ks[9], (DEPTH, N_HEADS, 2 * WIN_R - 1, 2 * WIN_C - 1), 0.1),
        'w_att_o': _normal(ks[10], (DEPTH, ATT_W, d), ATT_W ** -0.5),
        'w_fourier': _normal(ks[11], (DEPTH, FOURIER_W, d), FOURIER_W ** -0.5),
        'w_pool': _normal(ks[12], (DEPTH, len(POOL_WINDOWS), POOL_GROUP_W, POOL_OUT_GROUP), POOL_GROUP_W ** -0.5),
        'pool_scale': 1.0 + _normal(ks[13], (DEPTH, d), 0.02),
        'w_out': _normal(ks[14], (DEPTH, d, d), d ** -0.5),
        'w_router': _normal(ks[15], (DEPTH, d, N_EXPERTS), d ** -0.5),
        'w_exp_gate': _normal(ks[16], (DEPTH, N_EXPERTS, d, EXPERT_FF), d ** -0.5),
        'w_exp_up': _normal(ks[17], (DEPTH, N_EXPERTS, d, EXPERT_FF), d ** -0.5),
        'w_exp_down': _normal(ks[18], (DEPTH, N_EXPERTS, EXPERT_FF, d), EXPERT_FF ** -0.5),
        'final_norm_g': 1.0 + _normal(ks[19], (d,), 0.02),
    }


def reference(x, c, ctx, c_ctx, ada_w, ada_b, norm1_g, norm2_g, w_in, rpb, w_att_o, w_fourier,
              w_pool, pool_scale, w_out, w_router, w_exp_gate, w_exp_up, w_exp_down, final_norm_g):
    for i in range(DEPTH):
        x, ctx = trunk_layer(x, ctx, c, c_ctx, ada_w[i], ada_b[i], norm1_g[i], norm2_g[i], w_in[i], rpb[i],
                             w_att_o[i], w_fourier[i], w_pool[i], pool_scale[i], w_out[i], w_router[i],
                             w_exp_gate[i], w_exp_up[i], w_exp_down[i], update_ctx=(i < DEPTH - 1))
    return rmsnorm(x, final_norm_g)
```

```python
import numpy as np
from contextlib import ExitStack
import concourse.bass as bass
import concourse.mybir as mybir
from concourse.bass_utils import run_bass_kernel_spmd

F32 = mybir.dt.float32
F32R = mybir.dt.float32r
BF16 = mybir.dt.bfloat16
AF = mybir.ActivationFunctionType
ALU = mybir.AluOpType

D = 1024
T = 4096
TC = 256
DEPTH = 2
NH = 16
HD = 64
IN_W = 7168
K_OFF, V_OFF, Q_OFF, F_OFF, P_OFF, G_OFF = 0, 1024, 2048, 3072, 3584, 4096
NE = 16
FF = 2816
EPS = 1e-6
NEG = -30000.0
ENGS = ['pe', 'act', 'dve', 'pool', 'sp']


class Buf:
    __slots__ = ('name', 'last_w', 'readers', 'semslot', 'tracked')

    def __init__(self, name, tracked=True):
        self.name = name
        self.last_w = None
        self.readers = []
        self.semslot = None
        self.tracked = tracked


class V:
    __slots__ = ('buf', 'ap')

    def __init__(self, buf, ap):
        self.buf = buf
        self.ap = ap

    def __getitem__(self, idx):
        return V(self.buf, self.ap[idx])

    def r(self):
        return V(self.buf, self.ap.bitcast(F32R))

    def re(self, pat, **kw):
        return V(self.buf, self.ap.rearrange(pat, **kw))


class Op:
    __slots__ = ('eng', 'fn', 'kind', 'deps', 'signal', 'val', 'semslot', 'gidx', 'key')

    def __init__(self, eng, fn, kind):
        self.eng = eng
        self.fn = fn
        self.kind = kind
        self.deps = []
        self.signal = False
        self.val = None
        self.semslot = None
        self.gidx = 0
        self.key = 0


class Prog:
    def __init__(self, nc, stack, n_dma_sems=96):
        self.nc = nc
        self.eobj = {'pe': nc.tensor, 'act': nc.scalar, 'dve': nc.vector, 'pool': nc.gpsimd, 'sp': nc.sync}
        self.esem = {e: stack.enter_context(nc.semaphore('es_' + e)) for e in ENGS}
        self.ecount = {e: 0 for e in ENGS}
        self.slots = [[stack.enter_context(nc.semaphore('ds%d' % i)), 0] for i in range(n_dma_sems)]
        self.free_slots = list(range(n_dma_sems))
        self.ops = {e: [] for e in ENGS}
        self.waited = {e: {} for e in ENGS}
        self.phase_bufs = []
        self.pstack = None
        self.nphase = 0
        self.uid = 0
        self.gcount = 0

    def begin(self):
        self.pstack = ExitStack()
        self.pstack.__enter__()

    def _name(self, name):
        self.uid += 1
        return '%s_%d' % (name, self.uid)

    def tile(self, shape, dtype=F32, name='t', stack=None):
        st = stack or self.pstack
        h = st.enter_context(self.nc.sbuf_tensor(self._name(name), list(shape), dtype))
        b = Buf(name)
        self.phase_bufs.append(b)
        return V(b, h[:])

    def psum(self, shape=(128, 512), dtype=F32, name='ps'):
        h = self.pstack.enter_context(self.nc.psum_tensor(self._name(name), list(shape), dtype))
        b = Buf(name)
        self.phase_bufs.append(b)
        return V(b, h[:])

    def _record(self, op, reads, writes):
        deps = []
        for v in reads:
            b = v.buf
            if b.tracked and b.last_w is not None:
                deps.append(b.last_w)
        for v in writes:
            b = v.buf
            if b.tracked:
                if b.last_w is not None:
                    deps.append(b.last_w)
                deps.extend(b.readers)
        self.gcount += 1
        op.gidx = self.gcount
        for d in deps:
            if d is op:
                continue
            op.key = max(op.key, d.gidx if d.kind == 'c' else d.key)
            if d.kind == 'c':
                if d.eng == op.eng and d.eng == 'pe' and op.kind == 'c':
                    continue
                d.signal = True
            op.deps.append(d)
        for v in reads:
            if v.buf.tracked:
                v.buf.readers.append(op)
        for v in writes:
            if v.buf.tracked:
                v.buf.last_w = op
                v.buf.readers = []
        self.ops[op.eng].append(op)

    def op(self, eng, fn, reads=(), writes=()):
        o = Op(eng, fn, 'c')
        self._record(o, reads, writes)
        return o

    def dma(self, out, in_, q='sp', n=1, fn=None, **kw):
        sb = out.buf if out.buf.tracked else in_.buf
        assert sb.tracked
        if sb.semslot is None:
            sb.semslot = self.free_slots.pop()
        o = Op(q, None, 'd')
        o.semslot = sb.semslot
        slot = self.slots[sb.semslot]
        slot[1] += 16 * n
        o.val = slot[1]
        if fn is None:
            oap, iap = out.ap, in_.ap
            o.fn = lambda e: [e.dma_start(out=oap, in_=iap, **kw)]
        else:
            o.fn = fn
        self._record(o, [in_], [out])
        return o

    def dmar(self, out, in_, **kw):
        return self.dma(out.r(), in_.r(), **kw)

    def flush(self):
        nc = self.nc
        for e in ENGS:
            for o in self.ops[e]:
                if o.kind == 'c' and o.signal:
                    self.ecount[e] += 1
                    o.val = self.ecount[e]
        used_slots = set()
        for e in ENGS:
            for o in self.ops[e]:
                if o.kind == 'd':
                    used_slots.add(o.semslot)
        drain_eng = 'sp'
        with nc.Block() as block:
            secs = {'pe': block.tensor, 'act': block.scalar, 'dve': block.vector, 'pool': block.gpsimd,
                    'sp': block.sync}
            for e in ENGS:
                ops = self.ops[e]
                if not ops and e != drain_eng:
                    continue

                def body(eo, ops=ops, e=e):
                    waited = self.waited[e]
                    for o in ops:
                        for d in o.deps:
                            if d.kind == 'c':
                                key = ('e', d.eng)
                                sem = self.esem[d.eng]
                            else:
                                key = ('d', d.semslot)
                                sem = self.slots[d.semslot][0]
                            if waited.get(key, 0) >= d.val:
                                continue
                            eo.wait_ge(sem, d.val)
                            waited[key] = d.val
                        ins = o.fn(eo)
                        if o.kind == 'c':
                            if o.signal:
                                ins.then_inc(self.esem[e], 1)
                        else:
                            for i in ins:
                                i.then_inc(self.slots[o.semslot][0], 16)
                    if e == drain_eng:
                        for s in sorted(used_slots):
                            sem, cnt = self.slots[s]
                            if waited.get(('d', s), 0) < cnt:
                                eo.wait_ge(sem, cnt)
                                waited[('d', s)] = cnt

                secs[e](body)
        for b in self.phase_bufs:
            b.last_w = None
            b.readers = []
            if b.semslot is not None:
                self.free_slots.append(b.semslot)
                b.semslot = None
        self.phase_bufs = []
        self.ops = {e: [] for e in ENGS}
        self.pstack.__exit__(None, None, None)
        self.pstack = None
        self.nphase += 1

    def mm(self, out, lhsT, rhs, start, stop):
        o, l, r = out.ap, lhsT.ap, rhs.ap
        return self.op('pe', lambda e: e.matmul(o, l, r, start=start, stop=stop), [lhsT, rhs], [out])

    def transpose(self, out, in_, ident):
        o, i, d = out.ap, in_.ap, ident.ap
        return self.op('pe', lambda e: e.transpose(o, i, d), [in_, ident], [out])

    def act(self, out, in_, func, scale=1.0, bias=0.0, eng='act', extra_reads=()):
        o, i = out.ap, in_.ap
        sc = scale.ap if isinstance(scale, V) else scale
        bi = bias.ap if isinstance(bias, V) else bias
        rd = [in_] + [x for x in (scale, bias) if isinstance(x, V)] + list(extra_reads)
        return self.op('act', lambda e: e.activation(o, i, func, bias=bi, scale=sc), rd, [out])

    def ts(self, out, in0, s1, s2, op0, op1=None, eng='dve'):
        o, i = out.ap, in0.ap
        a1 = s1.ap if isinstance(s1, V) else s1
        a2 = s2.ap if isinstance(s2, V) else s2
        rd = [in0] + [x for x in (s1, s2) if isinstance(x, V)]
        if op1 is None:
            return self.op(eng, lambda e: e.tensor_scalar(o, i, a1, None, op0), rd, [out])
        return self.op(eng, lambda e: e.tensor_scalar(o, i, a1, a2, op0, op1), rd, [out])

    def tt(self, out, in0, in1, op, eng='dve'):
        o, a, b = out.ap, in0.ap, in1.ap
        return self.op(eng, lambda e: e.tensor_tensor(o, a, b, op), [in0, in1], [out])

    def stt(self, out, in0, scalar, in1, op0, op1):
        o, a, b = out.ap, in0.ap, in1.ap
        s = scalar.ap if isinstance(scalar, V) else scalar
        rd = [in0, in1] + ([scalar] if isinstance(scalar, V) else [])
        return self.op('dve', lambda e: e.scalar_tensor_tensor(o, a, s, b, op0, op1), rd, [out])

    def copy(self, out, in_, eng='dve'):
        o, i = out.ap, in_.ap
        if eng == 'act':
            return self.op('act', lambda e: e.copy(o, i), [in_], [out])
        return self.op(eng, lambda e: e.tensor_copy(o, i), [in_], [out])

    def recip(self, out, in_):
        o, i = out.ap, in_.ap
        return self.op('dve', lambda e: e.reciprocal(o, i), [in_], [out])

    def memset(self, out, val, eng='dve'):
        o = out.ap
        return self.op(eng, lambda e: e.memset(o, val), [], [out])


class Ring:
    def __init__(self, items):
        self.items = items
        self.i = 0

    def next(self):
        it = self.items[self.i % len(self.items)]
        self.i += 1
        return it


def dram(nc, name, shape, dtype=F32, kind='Internal'):
    h = nc.dram_tensor(name, list(shape), dtype, kind=kind)
    return V(Buf(name, tracked=False), h.ap())


class Net:
    def __init__(self, nc, stack, dbg=(), stop=None):
        self.nc = nc
        self.dbg = set(dbg)
        self.stop = stop
        self.p = Prog(nc, stack)
        self.stack = stack
        p = self.p
        self.in_shapes = {
            'xT': ([D, T], F32), 'c': ([1, D], F32), 'ctxT': ([D, TC], F32), 'c_ctx': ([1, D], F32),
            'ada_w': ([DEPTH, D, 6 * D], F32), 'ada_b': ([DEPTH, 6 * D], F32),
            'norm1_g': ([DEPTH, D], F32), 'norm2_g': ([DEPTH, D], F32), 'w_in': ([DEPTH, D, IN_W], F32),
            'biasx': ([DEPTH * NH, 20, 128, 512], F32), 'w_att_o': ([DEPTH, D, D], F32),
            'w_fourier': ([DEPTH, 512, D], F32), 'w_pool': ([DEPTH, 4, 128, 256], F32),
            'pool_scale': ([DEPTH, D], F32), 'w_out': ([DEPTH, D, D], F32), 'w_router': ([DEPTH, D, NE], F32),
            'w_g': ([DEPTH * NE, D, FF], F32), 'w_u': ([DEPTH * NE, D, FF], F32),
            'w_d': ([DEPTH * NE, FF, D], F32), 'final_g': ([1, D], F32),
            'dftc': ([T, T], BF16), 'dfts': ([T, T], BF16), 'dftc_c': ([TC, TC], BF16),
            'dfts_c': ([TC, TC], BF16), 'dft128': ([128, 256], F32), 'invcnt': ([4, T], F32),
            'invcnt_c': ([4, TC], F32), 'ident': ([128, 128], F32), 'iota512': ([128, 512], F32),
            'jcol': ([128, 4], F32)}
        self.ins = {}
        self.out = dram(nc, 'outT', [D, T], F32, 'ExternalOutput')
        self.scr = {}

    def __getattr__(self, name):
        d = self.__dict__
        if 'in_shapes' in d and name in d['in_shapes']:
            if name not in d['ins']:
                sh, dt = d['in_shapes'][name]
                d['ins'][name] = dram(d['nc'], name, sh, dt, 'ExternalInput')
            return d['ins'][name]
        raise AttributeError(name)

    def S(self, name, shape, dtype=F32):
        if name not in self.scr:
            kind = 'ExternalOutput' if name in self.dbg else 'Internal'
            self.scr[name] = dram(self.nc, name, shape, dtype, kind)
        return self.scr[name]

    def col_load(self, dst, src_row):
        p = self.p
        oap = dst.ap
        iap = src_row.ap.rearrange("o (k q) -> q (o k)", q=128)
        p.dma(dst, src_row, fn=lambda e: [e.dma_start(out=oap, in_=iap, allow_slow_non_contiguous=True)])

    def phase_adaln(self):
        p = self.p
        p.begin()
        mod = self.S('mod', [DEPTH * 2, 6 * D])
        craw = p.tile([128, 8, 2], F32, 'craw')
        s2 = p.tile([128, 8, 2], F32, 's2')
        self.col_load(craw[:, :, 0], self.c)
        self.col_load(craw[:, :, 1], self.c_ctx)
        p.act(s2, craw, AF.Silu)
        wb = Ring([p.tile([128, 8, 512], F32, 'adaw') for _ in range(6)])
        bb = Ring([p.tile([2, 512], F32, 'adab') for _ in range(6)])
        ob = Ring([p.tile([2, 512], F32, 'modo') for _ in range(2)])
        ps = Ring([p.psum([128, 512], F32, 'adaps') for _ in range(2)])
        lq = []

        def aload(i):
            l, nb = divmod(i, 12)
            w = wb.next()
            p.dma(w, self.ada_w[l, :, nb * 512:(nb + 1) * 512].re("(k q) n -> q k n", q=128))
            b = bb.next()
            p.dma(b, V(self.ada_b.buf, self.ada_b.ap[l:l + 1, nb * 512:(nb + 1) * 512].partition_broadcast(2)
                       .rearrange("p o n -> p (o n)")))
            lq.append((w, b))

        for i in range(4):
            aload(i)
        for l in range(DEPTH):
            for nb in range(12):
                i = l * 12 + nb
                if i + 4 < DEPTH * 12:
                    aload(i + 4)
                w, b = lq[i]
                acc = ps.next()
                for k in range(8):
                    p.mm(acc[0:2, :], s2[:, k, :], w[:, k, :], k == 0, k == 7)
                o = ob.next()
                p.tt(o, acc[0:2, :], b, ALU.add)
                p.dma(mod[2 * l:2 * l + 2, nb * 512:(nb + 1) * 512], o)
        p.flush()

    def load_modcols(self, l, si, seg, name):
        p = self.p
        mod = self.S('mod', [DEPTH * 2, 6 * D])
        t = p.tile([128, 8], F32, name)
        self.col_load(t, mod[2 * l + si:2 * l + si + 1, seg * D:(seg + 1) * D])
        return t

    def norm_consts(self, l, si, which):
        p = self.p
        base = 0 if which == 1 else 3
        sh = self.load_modcols(l, si, base + 0, 'sh')
        sc = self.load_modcols(l, si, base + 1, 'sc')
        g = p.tile([128, 8], F32, 'ng')
        ng = self.norm1_g if which == 1 else self.norm2_g
        self.col_load(g, ng[l:l + 1, :])
        gs = p.tile([128, 8], F32, 'gs')
        p.stt(gs, sc, 1.0, g, ALU.add, ALU.mult)
        return gs, sh

    def emit_norm(self, xsrc, t0, nt, gs, sh, hT, hoff, ones, xring, sqring, tmpring, psring, rsring):
        p = self.p
        xb = xring.next()
        p.dma(xb[:, :, 0:nt], xsrc[:, t0:t0 + nt].re("(k q) t -> q k t", q=128))
        acc = psring.next()
        for k in range(8):
            sq = sqring.next()
            p.act(sq[:, 0:nt], xb[:, k, 0:nt], AF.Square)
            p.mm(acc[:, 0:nt], ones, sq[:, 0:nt], k == 0, k == 7)
        rs = rsring.next()
        p.act(rs[:, 0:nt], acc[:, 0:nt], AF.Sqrt, scale=1.0 / D, bias=self.epsc)
        p.recip(rs[:, 0:nt], rs[:, 0:nt])
        for k in range(8):
            tmp = tmpring.next()
            p.tt(tmp[:, 0:nt], xb[:, k, 0:nt], rs[:, 0:nt], ALU.mult)
            p.act(hT[:, k, hoff:hoff + nt].r(), tmp[:, 0:nt], AF.Identity, scale=gs[:, k:k + 1], bias=sh[:, k:k + 1])

    def phase_normproj(self, l, xsrc, cxsrc, full_ctx):
        p = self.p
        dst = {}
        for si, Tn in ((0, T), (1, TC)):
            sfx = '%d_%d' % (l, si)
            d = {'k': self.S('kT' + sfx, [D, Tn], BF16), 'v': self.S('v' + sfx, [Tn, D], BF16)}
            if si == 0 or full_ctx:
                d['q'] = self.S('qT' + sfx, [D, Tn], BF16)
                d['u'] = self.S('uT' + sfx, [512, Tn])
                d['p'] = self.S('pT' + sfx, [512, Tn])
                d['g'] = self.S('gT' + sfx, [3 * D, Tn])
            dst[si] = d
        TBIG = 2048
        p.begin()
        cons = {0: self.norm_consts(l, 0, 1), 1: self.norm_consts(l, 1, 1)}
        srcs = {0: xsrc, 1: cxsrc}
        ones = p.tile([128, 128], F32, 'ones')
        p.memset(ones, 1.0)
        self.epsc = p.tile([128, 1], F32, 'epsc')
        p.memset(self.epsc, EPS)
        hT = p.tile([128, 8, TBIG + TC], F32, 'hT')
        xring = Ring([p.tile([128, 8, 512], F32, 'xb') for _ in range(2)])
        sqring = Ring([p.tile([128, 512], F32, 'sq') for _ in range(3)])
        tmpring = Ring([p.tile([128, 512], F32, 'tmp') for _ in range(3)])
        rsring = Ring([p.tile([128, 512], F32, 'rs') for _ in range(2)])
        psn = Ring([p.psum([128, 512], F32, 'psn') for _ in range(2)])
        psm = Ring([p.psum([128, 512], F32, 'psm') for _ in range(6)])
        wring = Ring([p.tile([128, 8, 512], F32, 'wblk') for _ in range(3)])
        o32 = Ring([p.tile([128, 512], F32, 'o32') for _ in range(8)])
        o16 = Ring([p.tile([128, 512], BF16, 'o16') for _ in range(10)])
        ev = 0
        passes = [[(0, 0, TBIG, 0), (1, 0, TC, TBIG)], [(0, TBIG, TBIG, 0)]]
        for segs in passes:
            for si, t0, n, ho in segs:
                nb = min(512, n)
                for bi in range(n // nb):
                    self.emit_norm(srcs[si], t0 + bi * nb, nb, cons[si][0], cons[si][1], hT, ho + bi * nb, ones,
                                   xring, sqring, tmpring, psn, rsring)
            wq = []

            def wload(cb):
                w = wring.next()
                p.dmar(w, self.w_in[l, :, cb * 512:(cb + 1) * 512].re("(k q) n -> q k n", q=128))
                wq.append(w)

            wload(0)
            wload(1)
            for cb in range(14):
                if cb + 2 < 14:
                    wload(cb + 2)
                w = wq[cb]
                c0 = cb * 512
                for si, t0, n, ho in segs:
                    d = dst[si]
                    if si == 1 and not full_ctx and c0 >= Q_OFF:
                        continue
                    nb = min(512, n)
                    if V_OFF <= c0 < Q_OFF:
                        for tt_ in range(n // 128):
                            acc = psm.next()
                            for k in range(8):
                                p.mm(acc, hT[:, k, ho + tt_ * 128:ho + (tt_ + 1) * 128].r(), w[:, k, :].r(),
                                     k == 0, k == 7)
                            o = o16.next()
                            ev += 1
                            p.copy(o, acc, eng='act' if ev % 2 else 'dve')
                            tok0 = t0 + tt_ * 128
                            p.dma(d['v'][tok0:tok0 + 128, c0 - V_OFF:c0 - V_OFF + 512], o)
                        continue
                    for ch in range(4):
                        col = c0 + ch * 128
                        for bi in range(n // nb):
                            acc = psm.next()
                            for k in range(8):
                                p.mm(acc[:, 0:nb], w[:, k, ch * 128:(ch + 1) * 128].r(),
                                     hT[:, k, ho + bi * nb:ho + (bi + 1) * nb].r(), k == 0, k == 7)
                            tok0 = t0 + bi * nb
                            ev += 1
                            if col < V_OFF:
                                o = o16.next()
                                p.copy(o[:, 0:nb], acc[:, 0:nb], eng='act' if ev % 2 else 'dve')
                                p.dma(d['k'][col:col + 128, tok0:tok0 + nb], o[:, 0:nb])
                            elif col < F_OFF:
                                o = o16.next()
                                if ev % 2:
                                    p.act(o[:, 0:nb], acc[:, 0:nb], AF.Copy, scale=HD ** -0.5)
                                else:
                                    p.ts(o[:, 0:nb], acc[:, 0:nb], HD ** -0.5, None, ALU.mult)
                                p.dma(d['q'][col - Q_OFF:col - Q_OFF + 128, tok0:tok0 + nb], o[:, 0:nb])
                            elif col < G_OFF:
                                o = o32.next()
                                p.copy(o[:, 0:nb], acc[:, 0:nb], eng='act' if ev % 2 else 'dve')
                                dd = d['u'] if col < P_OFF else d['p']
                                r0 = (col - F_OFF) % 512
                                p.dma(dd[r0:r0 + 128, tok0:tok0 + nb], o[:, 0:nb])
                            else:
                                o = o32.next()
                                p.act(o[:, 0:nb], acc[:, 0:nb], AF.Sigmoid)
                                p.dma(d['g'][col - G_OFF:col - G_OFF + 128, tok0:tok0 + nb], o[:, 0:nb])
        p.flush()

    def phase_attn(self, l, with_ctxq):
        p = self.p
        kT = self.S('kT%d_0' % l, [D, T], BF16)
        qT = self.S('qT%d_0' % l, [D, T], BF16)
        vv = self.S('v%d_0' % l, [T, D], BF16)
        kTc = self.S('kT%d_1' % l, [D, TC], BF16)
        vc = self.S('v%d_1' % l, [TC, D], BF16)
        attT = self.S('attT%d_0' % l, [D, T])
        if with_ctxq:
            qTc = self.S('qT%d_1' % l, [D, TC], BF16)
            attTc = self.S('attT%d_1' % l, [D, TC])
        p.begin()
        vall = p.tile([128, 32, D], BF16, 'vall')
        p.dma(vall, vv.re("(i q) d -> q i d", q=128))
        vcall = p.tile([128, 2, D], BF16, 'vcall')
        p.dma(vcall, vc.re("(i q) d -> q i d", q=128))
        idf = p.tile([128, 128], F32, 'idf')
        p.dma(idf, self.ident)
        idb = p.tile([128, 128], BF16, 'idb')
        p.copy(idb, idf)
        onesb = p.tile([128, 128], BF16, 'onesb2')
        p.memset(onesb, 1.0)
        khz = [p.tile([128, T], BF16, 'khz') for _ in range(2)]
        kcz = [p.tile([128, TC], BF16, 'kcz') for _ in range(2)]
        for par in range(2):
            z = slice(64, 128) if par == 0 else slice(0, 64)
            p.memset(khz[par][z, :], 0.0, eng='pool')
            p.memset(kcz[par][z, :], 0.0, eng='pool')
        qpr = Ring([p.tile([128, T], BF16, 'qp') for _ in range(2)])
        qcr = Ring([p.tile([128, TC], BF16, 'qcp') for _ in range(2)])
        bhr = Ring([p.tile([128, 20, 512], BF16, 'bh') for _ in range(2)])
        psS = Ring([p.psum([128, 512], F32, 'psS') for _ in range(4)])
        psN = Ring([p.psum([128, 512], F32, 'psN') for _ in range(2)])
        psD = Ring([p.psum([128, 512], F32, 'psD') for _ in range(2)])
        pr = Ring([p.tile([128, 512], BF16, 'pt') for _ in range(6)])
        rcr = Ring([p.tile([128, 512], F32, 'rc') for _ in range(2)])
        aor = Ring([p.tile([128, 512], F32, 'ao') for _ in range(3)])
        hq = {}

        def hload(h):
            hs = slice(h * 64, (h + 1) * 64)
            par = h % 2
            hp = slice(par * 64, par * 64 + 64)
            ps_ = slice((h // 2) * 128, (h // 2 + 1) * 128)
            p.dma(khz[par][hp, :], kT[hs, :])
            p.dma(kcz[par][hp, :], kTc[hs, :])
            if par == 0:
                qh = qpr.next()
                p.dma(qh, qT[ps_, :])
                qch = None
                if with_ctxq:
                    qch = qcr.next()
                    p.dma(qch, qTc[ps_, :])
                hq[h // 2] = (qh, qch)
            bh = bhr.next()
            p.dma(bh, self.biasx[l * NH + h].re("n q f -> q n f"), q='pool')
            hq[('b', h)] = bh

        hload(0)
        for h in range(NH):
            hs = slice(h * 64, (h + 1) * 64)
            par = h % 2
            hp = slice(par * 64, par * 64 + 64)
            ps_ = slice((h // 2) * 128, (h // 2 + 1) * 128)
            if h + 1 < NH:
                hload(h + 1)
            kh = khz[par]
            kch = kcz[par]
            qh, qch = hq[h // 2]
            bh = hq[('b', h)]
            jobs = [(j, 512) for j in range(8)] + ([(-1, TC)] if with_ctxq else [])
            for j, nq in jobs:
                if j >= 0:
                    tiles, base = chunk_tiles(j)
                    qv = qh[:, j * 512:(j + 1) * 512]
                else:
                    tiles, base = [], 0
                    qv = qch[:, 0:TC]
                num = psN.next()
                den = psD.next()
                ntot = len(tiles) + 2
                pend = []

                def pv(item, num=num, den=den, nq=nq, ntot=ntot):
                    ti, vt, pt = item
                    p.mm(num[:, 0:nq], vt, pt[:, 0:nq], ti == 0, ti == ntot - 1)
                    p.mm(den[:, 0:nq], onesb, pt[:, 0:nq], ti == 0, ti == ntot - 1)

                for ti in range(ntot):
                    S_ = psS.next()
                    if ti < len(tiles):
                        m = tiles[ti]
                        p.mm(S_[:, 0:nq], kh[:, m * 128:(m + 1) * 128], qv, True, False)
                        p.mm(S_[:, 0:nq], idb, bh[:, base + ti, :], False, True)
                        vt = vall[:, m, ps_]
                    else:
                        cm = ti - len(tiles)
                        p.mm(S_[:, 0:nq], kch[:, cm * 128:(cm + 1) * 128], qv, True, True)
                        vt = vcall[:, cm, ps_]
                    pt = pr.next()
                    p.act(pt[:, 0:nq], S_[:, 0:nq], AF.Exp)
                    pend.append((ti, vt, pt))
                    if len(pend) > 2:
                        pv(pend.pop(0))
                while pend:
                    pv(pend.pop(0))
                rc = rcr.next()
                p.recip(rc[hp, 0:nq], den[hp, 0:nq])
                ao = aor.next()
                p.tt(ao[hp, 0:nq], num[hp, 0:nq], rc[hp, 0:nq], ALU.mult)
                if j >= 0:
                    p.dma(attT[hs, j * 512:(j + 1) * 512], ao[hp, :])
                else:
                    p.dma(attTc[hs, 0:TC], ao[hp, 0:TC])
        p.flush()

    def phase_fourier_pool(self, l, si):
        p = self.p
        Tn = T if si == 0 else TC
        NI = Tn // 128
        NBk = min(1024, Tn)
        nh = (NBk + 511) // 512
        hw = min(512, NBk)
        sfx = '%d_%d' % (l, si)
        uT = self.S('uT' + sfx, [512, Tn])
        pT = self.S('pT' + sfx, [512, Tn])
        fourT = self.S('fourT' + sfx, [512, Tn])
        poolT = self.S('poolT' + sfx, [512, Tn])
        tabs = (self.dftc, self.dfts) if si == 0 else (self.dftc_c, self.dfts_c)
        icn = self.invcnt if si == 0 else self.invcnt_c
        p.begin()
        d128 = p.tile([128, 256], F32, 'd128')
        p.dmar(d128, self.dft128)
        AB = p.tile([128, NI, 4, 256], BF16, 'AB')
        ub = p.tile([128, Tn], F32, 'ub')
        acc = [[p.psum([128, 512], F32, 'facc') for _ in range(nh)] for _ in range(4)]
        ps1 = Ring([acc[0][0], acc[1][0]])
        pring = Ring([p.tile([128, 8, NBk], BF16, 'dpc') for _ in range(3)])
        o32 = Ring([p.tile([128, 512], F32, 'fo') for _ in range(4)])
        L = Tn + 32
        U = p.tile([128, L], F32, 'pU')
        A = p.tile([128, L], F32, 'pA')
        B = p.tile([128, L], F32, 'pB')
        iclo = p.tile([128, 16], F32, 'iclo')
        ichi = p.tile([128, 16], F32, 'ichi')
        for g in range(4):
            w = 2 << g
            half = w // 2
            eng = 'pool'
            p.memset(U[:, 0:16], 0.0, eng=eng)
            p.memset(U[:, 16 + Tn:L], 0.0, eng=eng)
            p.dma(U[:, 16:16 + Tn], pT[g * 128:(g + 1) * 128, :], q='pool')
            p.dma(iclo, V(icn.buf, icn.ap[g:g + 1, 0:16].partition_broadcast(128).rearrange("p o n -> p (o n)")), q='pool')
            p.dma(ichi, V(icn.buf, icn.ap[g:g + 1, Tn - 16:Tn].partition_broadcast(128)
                          .rearrange("p o n -> p (o n)")), q='pool')
            cur, n, bufs, bi = U, 1, [A, B], 0
            while n < w:
                nxt = bufs[bi]
                bi ^= 1
                ln = L - (2 * n - 1)
                p.tt(nxt[:, 0:ln], cur[:, 0:ln], cur[:, n:n + ln], ALU.add, eng=eng)
                cur = nxt
                n *= 2
            res = bufs[bi]
            s0 = 16 - half
            p.ts(res[:, 0:Tn], cur[:, s0:s0 + Tn], 1.0 / w, None, ALU.mult, eng=eng)
            p.tt(res[:, 0:16], cur[:, s0:s0 + 16], iclo, ALU.mult, eng=eng)
            p.tt(res[:, Tn - 16:Tn], cur[:, s0 + Tn - 16:s0 + Tn], ichi, ALU.mult, eng=eng)
            p.tt(res[:, 0:Tn], res[:, 0:Tn], U[:, 16:16 + Tn], ALU.subtract, eng=eng)
            p.dma(poolT[g * 128:(g + 1) * 128, :], res[:, 0:Tn], q='pool')
        ev = 0
        for g in range(4):
            p.dmar(ub, uT[g * 128:(g + 1) * 128, :])
            for i in range(NI):
                ps = ps1.next()
                p.mm(ps[:, 0:256], ub[:, i * 128:(i + 1) * 128].r(), d128.r(), True, True)
                p.copy(AB[:, i, g, 0:128], ps[:, 0:128], eng='act')
                p.ts(AB[:, i, g, 128:256], ps[:, 128:256], -1.0, None, ALU.mult)
        for kb in range(Tn // NBk):
            for ti, tbl in enumerate(tabs):
                for lg in range(max(1, NI // 8)):
                    nl = min(8, NI)
                    pc = pring.next()
                    p.dma(pc[:, 0:nl, :], tbl[lg * 1024:lg * 1024 + nl * 128, kb * NBk:(kb + 1) * NBk]
                          .re("(i q) k -> q i k", q=128))
                    for ii in range(nl):
                        i = lg * 8 + ii
                        for g in range(4):
                            for h in range(nh):
                                p.mm(acc[g][h][:, 0:hw], AB[:, i, g, ti * 128:(ti + 1) * 128],
                                     pc[:, ii, h * 512:h * 512 + hw], ti == 0 and i == 0, ti == 1 and i == NI - 1)
            for g in range(4):
                for h in range(nh):
                    o = o32.next()
                    ev += 1
                    p.copy(o[:, 0:hw], acc[g][h][:, 0:hw], eng='act' if ev % 2 else 'dve')
                    c0 = kb * NBk + h * 512
                    p.dma(fourT[g * 128:(g + 1) * 128, c0:c0 + hw], o[:, 0:hw])
        p.flush()

    def phase_merge(self, l, si, xsrc, xdst):
        p = self.p
        Tn = T if si == 0 else TC
        NB = min(512, Tn)
        sfx = '%d_%d' % (l, si)
        attT = self.S('attT' + sfx, [D, Tn])
        fourT = self.S('fourT' + sfx, [512, Tn])
        poolT = self.S('poolT' + sfx, [512, Tn])
        gT = self.S('gT' + sfx, [3 * D, Tn])
        p.begin()
        wa = p.tile([128, 8, D], F32, 'wa')
        p.dmar(wa, self.w_att_o[l].re("(k q) n -> q k n", q=128))
        wf = p.tile([128, 4, D], F32, 'wf')
        p.dmar(wf, self.w_fourier[l].re("(k q) n -> q k n", q=128))
        wp = p.tile([128, 4, 256], F32, 'wp')
        p.dmar(wp, self.w_pool[l].re("g c f -> c g f"))
        wo = p.tile([128, 8, D], F32, 'wo')
        p.dmar(wo, self.w_out[l].re("(k q) n -> q k n", q=128))
        g1 = self.load_modcols(l, si, 2, 'g1')
        psc = p.tile([128, 8], F32, 'psc')
        self.col_load(psc, self.pool_scale[l:l + 1, :])
        at = p.tile([128, 8, NB], F32, 'at')
        fo = p.tile([128, 4, NB], F32, 'fo')
        po = p.tile([128, 4, NB], F32, 'po')
        xbr = Ring([p.tile([128, 8, NB], F32, 'xb') for _ in range(2)])
        mg = p.tile([128, 8, NB], F32, 'mg')
        gring = Ring([p.tile([128, 3, NB], F32, 'gt') for _ in range(2)])
        tmp = Ring([p.tile([128, NB], F32, 'mt') for _ in range(6)])
        o32 = Ring([p.tile([128, NB], F32, 'xo') for _ in range(2)])
        psa = Ring([p.psum([128, 512], F32, 'psa') for _ in range(2)])
        psf = Ring([p.psum([128, 512], F32, 'psf') for _ in range(2)])
        psp = Ring([p.psum([128, 512], F32, 'psp') for _ in range(2)])
        pso = Ring([p.psum([128, 512], F32, 'pso') for _ in range(2)])
        gview = gT.re("(b k q) t -> q b k t", b=3, q=128)
        def load_in(tb):
            t0 = tb * NB
            p.dmar(at, attT[:, t0:t0 + NB].re("(k q) t -> q k t", q=128))
            p.dmar(fo, fourT[:, t0:t0 + NB].re("(k q) t -> q k t", q=128))
            p.dmar(po, poolT[:, t0:t0 + NB].re("(k q) t -> q k t", q=128))

        load_in(0)
        for tb in range(Tn // NB):
            t0 = tb * NB
            xb = xbr.next()
            p.dma(xb, xsrc[:, t0:t0 + NB].re("(k q) t -> q k t", q=128))
            for dc in range(8):
                cs = slice(dc * 128, (dc + 1) * 128)
                gt = gring.next()
                p.dma(gt, gview[:, :, dc, t0:t0 + NB])
                ya = psa.next()
                for k in range(8):
                    p.mm(ya[:, 0:NB], wa[:, k, cs].r(), at[:, k, :].r(), k == 0, k == 7)
                yf = psf.next()
                for k in range(4):
                    p.mm(yf[:, 0:NB], wf[:, k, cs].r(), fo[:, k, :].r(), k == 0, k == 3)
                yp = psp.next()
                p.mm(yp[:, 0:NB], wp[:, dc // 2, (dc % 2) * 128:(dc % 2 + 1) * 128].r(), po[:, dc // 2, :].r(),
                     True, True)
                m1, m2, m3 = tmp.next(), tmp.next(), tmp.next()
                p.tt(m1, ya[:, 0:NB], gt[:, 0, :], ALU.mult)
                p.tt(m2, yf[:, 0:NB], gt[:, 1, :], ALU.mult)
                p.stt(m3, yp[:, 0:NB], psc[:, dc:dc + 1], gt[:, 2, :], ALU.mult, ALU.mult)
                p.tt(m1, m1, m2, ALU.add, eng='pool')
                p.tt(mg[:, dc, :].r(), m1, m3, ALU.add)
            if tb + 1 < Tn // NB:
                load_in(tb + 1)
            for dc in range(8):
                cs = slice(dc * 128, (dc + 1) * 128)
                yo = pso.next()
                for k in range(8):
                    p.mm(yo[:, 0:NB], wo[:, k, cs].r(), mg[:, k, :].r(), k == 0, k == 7)
                xo = o32.next()
                p.stt(xo, yo[:, 0:NB], g1[:, dc:dc + 1], xb[:, dc, :], ALU.mult, ALU.add)
                p.dma(xdst[cs, t0:t0 + NB], xo)
        p.flush()

    def phase_route(self, l, si, xsrc):
        p = self.p
        Tn = T if si == 0 else TC
        NI = Tn // 128
        NB = min(512, Tn)
        cap = 2 * Tn // NE
        sfx = '%d_%d' % (l, si)
        h2 = self.S('h2tok' + sfx, [Tn, D], BF16)
        posT = self.S('posT' + sfx, [NE, Tn])
        affTd = self.S('affT' + sfx, [NE, Tn])
        postm = self.S('postm' + sfx, [128, NI * NE])
        p.begin()
        gs, sh = self.norm_consts(l, si, 2)
        ones = p.tile([128, 128], F32, 'ones')
        p.memset(ones, 1.0)
        self.epsc = p.tile([128, 1], F32, 'epsc')
        p.memset(self.epsc, EPS)
        idf = p.tile([128, 128], F32, 'idf')
        p.dma(idf, self.ident)
        wr = p.tile([128, 8, NE], F32, 'wr')
        p.dma(wr, self.w_router[l].re("(k q) e -> q k e", q=128))
        eT = p.tile([NE, Tn], F32, 'eT')
        affT = p.tile([NE, Tn], F32, 'affT')
        hring = Ring([p.tile([128, 8, NB], F32, 'h2T') for _ in range(2)])
        xring = Ring([p.tile([128, 8, NB], F32, 'xb') for _ in range(2)])
        sqring = Ring([p.tile([128, NB], F32, 'sq') for _ in range(3)])
        tmpring = Ring([p.tile([128, NB], F32, 'tmp') for _ in range(3)])
        rsring = Ring([p.tile([128, NB], F32, 'rs') for _ in range(2)])
        htr = Ring([p.tile([128, D], BF16, 'htok') for _ in range(3)])
        psn = Ring([p.psum([128, 512], F32, 'psn') for _ in range(2)])
        psl = Ring([p.psum([128, 512], F32, 'psl') for _ in range(2)])
        pst = Ring([p.psum([128, 512], F32, 'pst') for _ in range(4)])
        ev = 0
        for bi in range(Tn // NB):
            hT = hring.next()
            self.emit_norm(xsrc, bi * NB, NB, gs, sh, hT, 0, ones, xring, sqring, tmpring, psn, rsring)
            lg = psl.next()
            for k in range(8):
                p.mm(lg[0:NE, 0:NB], wr[:, k, :], hT[:, k, :], k == 0, k == 7)
            p.act(eT[:, bi * NB:(bi + 1) * NB], lg[0:NE, 0:NB], AF.Exp)
            for tt_ in range(NB // 128):
                ht = htr.next()
                for hf in range(2):
                    pt = pst.next()
                    for kk in range(4):
                        k = hf * 4 + kk
                        p.transpose(pt[:, kk * 128:(kk + 1) * 128], hT[:, k, tt_ * 128:(tt_ + 1) * 128], idf)
                    ev += 1
                    p.copy(ht[:, hf * 512:(hf + 1) * 512], pt, eng='act' if ev % 2 else 'dve')
                tok0 = bi * NB + tt_ * 128
                p.dma(h2[tok0:tok0 + 128, :], ht)
        rcs = Ring([p.tile([NE, NB], F32, 'rcs') for _ in range(2)])
        for bi in range(Tn // NB):
            bs = slice(bi * NB, (bi + 1) * NB)
            sm = psl.next()
            p.mm(sm[0:NE, 0:NB], ones[0:NE, 0:NE], eT[:, bs], True, True)
            rc = rcs.next()
            p.recip(rc, sm[0:NE, 0:NB])
            p.tt(affT[:, bs], eT[:, bs], rc, ALU.mult)
        lo = p.tile([NE, 1], F32, 'lo')
        hi = p.tile([NE, 1], F32, 'hi')
        mid = p.tile([NE, 1], F32, 'mid')
        cnt = p.tile([NE, 1], F32, 'cnt')
        ge = p.tile([NE, 1], F32, 'ge')
        d1 = p.tile([NE, 1], F32, 'd1')
        junk = p.tile([NE, Tn], F32, 'junk')
        p.memset(lo, 0.0)
        p.memset(hi, 1.0)
        for it in range(30):
            p.ts(mid, lo, hi, 0.5, ALU.add, ALU.mult)
            ja, aa, ma, ca = junk.ap, affT.ap, mid.ap, cnt.ap
            p.op('dve', lambda e, ja=ja, aa=aa, ma=ma, ca=ca: e.tensor_scalar(ja, aa, ma, None, ALU.is_ge, ALU.add,
                                                                               accum_out=ca),
                 [affT, mid], [junk, cnt])
            p.ts(ge, cnt, float(cap), None, ALU.is_ge)
            p.tt(d1, mid, lo, ALU.subtract)
            p.stt(lo, d1, ge, lo, ALU.mult, ALU.add)
            p.tt(d1, hi, mid, ALU.subtract)
            p.stt(hi, d1, ge, mid, ALU.mult, ALU.add)
        mask = p.tile([NE, Tn], F32, 'mask')
        p.ts(mask, affT, lo, None, ALU.is_ge)
        p.memset(junk, 1.0)
        cum = p.tile([NE, Tn], F32, 'cum')
        ca, ja, ma = cum.ap, junk.ap, mask.ap
        p.op('dve', lambda e: e.tensor_tensor_scan(ca, ja, ma, 0.0, ALU.mult, ALU.add), [junk, mask], [cum])
        p.tt(cum, cum, mask, ALU.mult)
        p.ts(cum, cum, -1.0, None, ALU.add)
        p.dma(posT, cum)
        p.dma(affTd, affT)
        ptm = p.tile([128, NI * NE], F32, 'ptm')
        pt = pst.next()
        for i in range(NI):
            p.transpose(pt[:, i * NE:(i + 1) * NE], cum[:, i * 128:(i + 1) * 128], idf[0:NE, 0:NE])
        p.copy(ptm, pt[:, 0:NI * NE])
        p.dma(postm, ptm)
        p.flush()

    def phase_gather(self, l, streams):
        p = self.p
        p.begin()
        iota = p.tile([128, 512], F32, 'iota')
        p.dma(iota, self.iota512)
        acc = [p.psum([128, 512], F32, 'gacc') for _ in range(8)]
        sring = Ring([p.tile([128, 512], BF16, 'S') for _ in range(4)])
        xor_ = Ring([p.tile([128, 8, 512], F32, 'xgo') for _ in range(2)])
        ev = 0
        for si in streams:
            Tn = T if si == 0 else TC
            NI = Tn // 128
            cap = 2 * Tn // NE
            sfx = '%d_%d' % (l, si)
            h2 = self.S('h2tok' + sfx, [Tn, D], BF16)
            postm = self.S('postm' + sfx, [128, NI * NE])
            xg = self.S('xg' + sfx, [NE, D, cap])
            h2tok = p.tile([128, NI, D], BF16, 'h2tok')
            p.dma(h2tok, h2.re("(i q) d -> q i d", q=128))
            ptm = p.tile([128, NI, NE], F32, 'ptm')
            p.dma(ptm, postm.re("q (i e) -> q i e", e=NE))
            for e in range(NE):
                for i in range(NI):
                    S_ = sring.next()
                    p.ts(S_[:, 0:cap], iota[:, 0:cap], ptm[:, i, e:e + 1], None, ALU.is_equal)
                    for k in range(8):
                        p.mm(acc[k][:, 0:cap], h2tok[:, i, k * 128:(k + 1) * 128], S_[:, 0:cap], i == 0, i == NI - 1)
                xo = xor_.next()
                for k in range(8):
                    ev += 1
                    p.copy(xo[:, k, 0:cap], acc[k][:, 0:cap], eng='act' if ev % 2 else 'dve')
                p.dma(xg[e].re("(k q) c -> q k c", q=128), xo[:, :, 0:cap])
        p.flush()

    def phase_experts(self, l, streams):
        p = self.p
        p.begin()
        info = {}
        for si in streams:
            Tn = T if si == 0 else TC
            cap = 2 * Tn // NE
            Mp = cap if cap >= 128 else 128
            sfx = '%d_%d' % (l, si)
            xg = self.S('xg' + sfx, [NE, D, cap])
            ysc = self.S('y' + sfx, [NE, Mp, D], BF16)
            hT = p.tile([128, FF // 128, Mp], F32, 'ehT')
            if Mp != cap:
                p.memset(hT, 0.0)
            xr = Ring([p.tile([128, 8, cap], F32, 'exg') for _ in range(2)])
            yb = [p.psum([128, 512], F32, 'ey') for _ in range(Mp // 128)]
            info[si] = (cap, Mp, xg, ysc, hT, xr, yb)
        wgr = Ring([p.tile([128, 8, 512], F32, 'wg') for _ in range(2)])
        wur = Ring([p.tile([128, 8, 512], F32, 'wu') for _ in range(2)])
        wdr = Ring([p.tile([128, 4, 512], F32, 'wd') for _ in range(3)])
        npa = 8 - sum(len(info[si][6]) for si in streams)
        psa = Ring([p.psum([128, 512], F32, 'ea') for _ in range(max(1, npa - npa // 2))])
        psu = Ring([p.psum([128, 512], F32, 'eu') for _ in range(max(1, npa // 2))])
        slr = Ring([p.tile([128, 512], F32, 'sl') for _ in range(2)])
        yor = Ring([p.tile([128, 512], BF16, 'yo') for _ in range(5)])
        NFB = (FF + 511) // 512
        ev = 0
        xsq, guq, dq = {}, {}, {}

        def xs_load(e):
            if e >= NE or e in xsq:
                return
            xs = {}
            for si in streams:
                cap, Mp, xg, ysc, hT, xr, yb = info[si]
                xs[si] = xr.next()
                p.dmar(xs[si], xg[e].re("(k q) c -> q k c", q=128))
            xsq[e] = xs

        def gu_load(e, fb):
            if e >= NE or fb >= NFB or (e, fb) in guq:
                return
            ge = l * NE + e
            f0 = fb * 512
            fw = min(512, FF - f0)
            wgb = wgr.next()
            p.dmar(wgb[:, :, 0:fw], self.w_g[ge, :, f0:f0 + fw].re("(k q) f -> q k f", q=128))
            wub = wur.next()
            p.dmar(wub[:, :, 0:fw], self.w_u[ge, :, f0:f0 + fw].re("(k q) f -> q k f", q=128))
            guq[(e, fb)] = (wgb, wub)

        def d_load(e, dh, fb):
            if e >= NE or fb >= NFB or (e, dh, fb) in dq:
                return
            ge = l * NE + e
            f0 = fb * 512
            fw = min(512, FF - f0)
            wdb = wdr.next()
            p.dmar(wdb[:, 0:fw // 128, :], self.w_d[ge, f0:f0 + fw, dh * 512:(dh + 1) * 512]
                   .re("(c q) d -> q c d", q=128))
            dq[(e, dh, fb)] = wdb

        xs_load(0)
        gu_load(0, 0)
        for e in range(NE):
            xs = xsq[e]
            for fb in range(NFB):
                fw = min(512, FF - fb * 512)
                gu_load(e, fb)
                gu_load(e, fb + 1)
                wgb, wub = guq[(e, fb)]
                for fcl in range(fw // 128):
                    fc = fb * 4 + fcl
                    fs = slice(fcl * 128, (fcl + 1) * 128)
                    for si in streams:
                        cap, Mp, xg, ysc, hT, xr, yb = info[si]
                        a = psa.next()
                        for k in range(8):
                            p.mm(a[:, 0:cap], wgb[:, k, fs].r(), xs[si][:, k, :].r(), k == 0, k == 7)
                        u = psu.next()
                        for k in range(8):
                            p.mm(u[:, 0:cap], wub[:, k, fs].r(), xs[si][:, k, :].r(), k == 0, k == 7)
                        sl = slr.next()
                        p.act(sl[:, 0:cap], a[:, 0:cap], AF.Silu)
                        p.tt(hT[:, fc, 0:cap].r(), u[:, 0:cap], sl[:, 0:cap], ALU.mult)
                if fb == NFB - 2:
                    d_load(e, 0, 0)
            for dh in range(2):
                for fb in range(NFB):
                    fw = min(512, FF - fb * 512)
                    d_load(e, dh, fb)
                    d_load(e, dh, fb + 1)
                    wdb = dq[(e, dh, fb)]
                    for fcl in range(fw // 128):
                        fc = fb * 4 + fcl
                        for si in streams:
                            cap, Mp, xg, ysc, hT, xr, yb = info[si]
                            for sc in range(Mp // 128):
                                p.mm(yb[sc], hT[:, fc, sc * 128:(sc + 1) * 128].r(), wdb[:, fcl, :].r(),
                                     fc == 0, fc == FF // 128 - 1)
                if dh == 0:
                    d_load(e, 1, 0)
                    d_load(e, 1, 1)
                else:
                    xs_load(e + 1)
                    gu_load(e + 1, 0)
                    gu_load(e + 1, 1)
                for si in streams:
                    cap, Mp, xg, ysc, hT, xr, yb = info[si]
                    for sc in range(Mp // 128):
                        o = yor.next()
                        ev += 1
                        p.copy(o, yb[sc], eng='act' if ev % 2 else 'dve')
                        p.dma(ysc[e, sc * 128:(sc + 1) * 128, dh * 512:(dh + 1) * 512], o)
        p.flush()

    def phase_combine(self, l, si, xsrc, xdst):
        p = self.p
        Tn = T if si == 0 else TC
        NB = min(512, Tn)
        cap = 2 * Tn // NE
        Mp = cap if cap >= 128 else 128
        JC = Mp // 128
        sfx = '%d_%d' % (l, si)
        ysc = self.S('y' + sfx, [NE, Mp, D], BF16)
        posT = self.S('posT' + sfx, [NE, Tn])
        affTd = self.S('affT' + sfx, [NE, Tn])
        p.begin()
        g2 = self.load_modcols(l, si, 5, 'g2')
        jcol = p.tile([128, 4], F32, 'jcol')
        p.dma(jcol, self.jcol)
        yh = [p.tile([128, JC, 512], BF16, 'yh') for _ in range(NE)]
        posr = [p.tile([128, 8, NB], F32, 'posb') for _ in range(2)]
        affr = [p.tile([128, 8, NB], F32, 'affb') for _ in range(2)]
        xbr = Ring([p.tile([128, 4, NB], F32, 'xb') for _ in range(2)])
        sgr = Ring([p.tile([128, NB], BF16, 'sg') for _ in range(4)])
        o32 = Ring([p.tile([128, NB], F32, 'xo') for _ in range(4)])
        accs = Ring([[p.psum([128, 512], F32, 'cacc') for _ in range(4)] for _ in range(2)])
        yv = ysc.re("e (j q) d -> q e j d", q=128)
        ntb = Tn // NB
        steps = [(dh, tb) for dh in range(2) for tb in range(ntb)]

        def load_pa(step, eh):
            dh, tb = steps[step]
            t0 = tb * NB
            es = slice(eh * 8, (eh + 1) * 8)
            p.dma(posr[eh], V(posT.buf, posT.ap[es, t0:t0 + NB].partition_broadcast(128)))
            p.dma(affr[eh], V(affTd.buf, affTd.ap[es, t0:t0 + NB].partition_broadcast(128)))

        load_pa(0, 0)
        load_pa(0, 1)
        for st, (dh, tb) in enumerate(steps):
            t0 = tb * NB
            if tb == 0:
                for e in range(NE):
                    p.dma(yh[e], yv[:, e, :, dh * 512:(dh + 1) * 512])
            xb = xbr.next()
            p.dma(xb, xsrc[dh * 512:(dh + 1) * 512, t0:t0 + NB].re("(k q) t -> q k t", q=128))
            acc = accs.next()
            n = NE * JC
            idx = 0
            for eh in range(2):
                for e8 in range(8):
                    e = eh * 8 + e8
                    for jc in range(JC):
                        sg = sgr.next()
                        p.stt(sg, posr[eh][:, e8, :], jcol[:, jc:jc + 1], affr[eh][:, e8, :], ALU.is_equal,
                              ALU.mult)
                        for dc in range(4):
                            p.mm(acc[dc][:, 0:NB], yh[e][:, jc, dc * 128:(dc + 1) * 128], sg, idx == 0, idx == n - 1)
                        idx += 1
                if st + 1 < len(steps):
                    load_pa(st + 1, eh)
            for dc in range(4):
                c = dh * 4 + dc
                xo = o32.next()
                p.stt(xo, acc[dc][:, 0:NB], g2[:, c:c + 1], xb[:, dc, :], ALU.mult, ALU.add)
                p.dma(xdst[c * 128:(c + 1) * 128, t0:t0 + NB], xo)
        p.flush()

    def phase_final(self, xsrc):
        p = self.p
        p.begin()
        gs = p.tile([128, 8], F32, 'fg')
        self.col_load(gs, self.final_g)
        sh = p.tile([128, 8], F32, 'fsh')
        p.memset(sh, 0.0)
        ones = p.tile([128, 128], F32, 'ones')
        p.memset(ones, 1.0)
        self.epsc = p.tile([128, 1], F32, 'epsc')
        p.memset(self.epsc, EPS)
        NB = 512
        hring = Ring([p.tile([128, 8, NB], F32, 'oT') for _ in range(2)])
        xring = Ring([p.tile([128, 8, NB], F32, 'xb') for _ in range(2)])
        sqring = Ring([p.tile([128, NB], F32, 'sq') for _ in range(3)])
        tmpring = Ring([p.tile([128, NB], F32, 'tmp') for _ in range(3)])
        rsring = Ring([p.tile([128, NB], F32, 'rs') for _ in range(2)])
        psn = Ring([p.psum([128, 512], F32, 'psn') for _ in range(2)])
        for bi in range(T // NB):
            hT = hring.next()
            self.emit_norm(xsrc, bi * NB, NB, gs, sh, hT, 0, ones, xring, sqring, tmpring, psn, rsring)
            p.dma(self.out[:, bi * NB:(bi + 1) * NB].re("(k q) t -> q k t", q=128), hT)
        p.flush()

    def build(self):
        self.phase_adaln()
        if self.stop == 'adaln':
            return
        x, cx = self.xT, self.ctxT
        for l in range(DEPTH):
            fc = l < DEPTH - 1
            st = lambda s: self.stop == '%s%d' % (s, l)
            self.phase_normproj(l, x, cx, fc)
            if st('proj'):
                return
            self.phase_attn(l, fc)
            if st('attn'):
                return
            self.phase_fourier_pool(l, 0)
            if fc:
                self.phase_fourier_pool(l, 1)
            if st('four'):
                return
            xm = self.S('xm%d' % l, [D, T])
            cm = self.S('cm%d' % l, [D, TC])
            self.phase_merge(l, 0, x, xm)
            if fc:
                self.phase_merge(l, 1, cx, cm)
            if st('merge'):
                return
            self.phase_route(l, 0, xm)
            if fc:
                self.phase_route(l, 1, cm)
            if st('route'):
                return
            streams = [0, 1] if fc else [0]
            self.phase_gather(l, streams)
            if st('gather'):
                return
            self.phase_experts(l, streams)
            if st('experts'):
                return
            xe = self.S('xe%d' % l, [D, T])
            ce = self.S('ce%d' % l, [D, TC])
            self.phase_combine(l, 0, xm, xe)
            if fc:
                self.phase_combine(l, 1, cm, ce)
            if st('combine'):
                return
            x, cx = xe, ce
        self.phase_final(x)


def build_nc(dbg=(), stop=None):
    nc = bass.Bass("TRN2", target_bir_lowering=False)
    nc.dge_precook = False
    stack = ExitStack()
    with stack:
        net = Net(nc, stack, dbg=dbg, stop=stop)
        net.build()
    return nc, net


def chunk_tiles(j):
    if j == 0:
        return list(range(0, 6)), 0
    if j == 7:
        return list(range(26, 32)), 14
    return list(range(4 * j - 2, 4 * j + 6)), 6


_CONSTS = None


def make_consts():
    global _CONSTS
    if _CONSTS is not None:
        return _CONSTS
    import ml_dtypes
    out = {}
    for name, L in (('', T), ('_c', TC)):
        idx = (np.arange(L, dtype=np.int64)[:, None] * np.arange(L, dtype=np.int64)[None, :]) % L
        base = 2.0 * np.pi * np.arange(L, dtype=np.float64) / L
        out['dftc' + name] = (np.cos(base)[idx] / np.sqrt(L)).astype(np.float32).astype(ml_dtypes.bfloat16)
        out['dfts' + name] = (np.sin(base)[idx] / np.sqrt(L)).astype(np.float32).astype(ml_dtypes.bfloat16)
        t = np.arange(L)
        ic = np.zeros((4, L), np.float32)
        for g, w in enumerate((2, 4, 8, 16)):
            lo = np.clip(t - w // 2, 0, L)
            hi = np.clip(t + w - w // 2, 0, L)
            ic[g] = 1.0 / (hi - lo)
        out['invcnt' + name] = ic
    ang = 2.0 * np.pi * ((np.arange(128)[:, None] * np.arange(128)[None, :]) % 128) / 128.0
    out['dft128'] = (np.concatenate([np.cos(ang), np.sin(ang)], axis=1) / np.sqrt(128.0)).astype(np.float32)
    out['ident'] = np.eye(128, dtype=np.float32)
    out['iota512'] = np.ascontiguousarray(np.broadcast_to(np.arange(512, dtype=np.float32), (128, 512)))
    out['jcol'] = (np.arange(4, dtype=np.float32)[None, :] * 128 + np.arange(128, dtype=np.float32)[:, None])
    _CONSTS = out
    return out


def make_biasx(rpb):
    a = np.arange(2)[:, None, None, None]
    kc = np.arange(64)[None, :, None, None]
    rl = np.arange(8)[None, None, :, None]
    c = np.arange(64)[None, None, None, :]
    cs = np.clip(c - 8, 0, 48)
    colok = (kc >= cs) & (kc < cs + 16)
    coff = np.clip(kc - c + 15, 0, 30)
    out = np.full((DEPTH, NH, 20, 2, 64, 8, 64), NEG, np.float32)
    for j in (0, 1, 7):
        tiles, base = chunk_tiles(j)
        for ti, m in enumerate(tiles):
            kr = 2 * m + a
            r = 8 * j + rl
            rs = np.clip(r - 4, 0, 56)
            rowok = (kr >= rs) & (kr <= rs + 7)
            roff = np.clip(kr - r + 7, 0, 14)
            ok = np.broadcast_to(rowok & colok, (2, 64, 8, 64))
            ro = np.broadcast_to(roff, (2, 64, 8, 64))
            co = np.broadcast_to(coff, (2, 64, 8, 64))
            vals = rpb[:, :, ro, co]
            out[:, :, base + ti] = np.where(ok[None, None], vals, np.float32(NEG))
    return np.ascontiguousarray(out.reshape(DEPTH * NH, 20, 128, 512))


def host_shared(inputs):
    sh = dict(make_consts())
    f = lambda k: np.ascontiguousarray(np.asarray(inputs[k], np.float32))
    sh['c_ctx'] = f('c_ctx').reshape(1, D)
    for k in ('ada_w', 'ada_b', 'norm1_g', 'norm2_g', 'w_in', 'w_att_o', 'w_fourier', 'w_pool', 'pool_scale',
              'w_out', 'w_router'):
        sh[k] = f(k)
    sh['w_g'] = f('w_exp_gate').reshape(DEPTH * NE, D, FF)
    sh['w_u'] = f('w_exp_up').reshape(DEPTH * NE, D, FF)
    sh['w_d'] = f('w_exp_down').reshape(DEPTH * NE, FF, D)
    sh['final_g'] = f('final_norm_g').reshape(1, D)
    sh['biasx'] = make_biasx(f('rpb'))
    return sh


def core_inputs(inputs, b, shared, names):
    m = {}
    for n in names:
        if n == 'xT':
            m[n] = np.ascontiguousarray(np.asarray(inputs['x'][b], np.float32).T)
        elif n == 'ctxT':
            m[n] = np.ascontiguousarray(np.asarray(inputs['ctx'][b], np.float32).T)
        elif n == 'c':
            m[n] = np.ascontiguousarray(np.asarray(inputs['c'][b], np.float32)).reshape(1, D)
        else:
            m[n] = shared[n]
    return m


def kernel(**inputs):
    nc, net = build_nc()
    shared = host_shared(inputs)
    names = list(net.ins.keys())
    in_maps = [core_inputs(inputs, b, shared, names) for b in range(8)]
    res = run_bass_kernel_spmd(nc, in_maps, core_ids=list(range(8)))
    out = np.stack([np.ascontiguousarray(r['outT'].T) for r in res.results], axis=0)
    return out.astype(np.float32)
```

```python
import numpy as np
from contextlib import ExitStack
import concourse.bass as bass
import concourse.mybir as mybir
from concourse.bass_utils import run_bass_kernel_spmd

F32 = mybir.dt.float32
F32R = mybir.dt.float32r
BF16 = mybir.dt.bfloat16
AF = mybir.ActivationFunctionType
ALU = mybir.AluOpType

D = 1024
T = 4096
TC = 256
DEPTH = 2
NH = 16
HD = 64
IN_W = 7168
K_OFF, V_OFF, Q_OFF, F_OFF, P_OFF, G_OFF = 0, 1024, 2048, 3072, 3584, 4096
NE = 16
FF = 2816
EPS = 1e-6
NEG = -30000.0
ENGS = ['pe', 'act', 'dve', 'pool', 'sp']


class Buf:
    __slots__ = ('name', 'last_w', 'readers', 'semslot', 'tracked')

    def __init__(self, name, tracked=True):
        self.name = name
        self.last_w = None
        self.readers = []
        self.semslot = None
        self.tracked = tracked


class V:
    __slots__ = ('buf', 'ap')

    def __init__(self, buf, ap):
        self.buf = buf
        self.ap = ap

    def __getitem__(self, idx):
        return V(self.buf, self.ap[idx])

    def r(self):
        return V(self.buf, self.ap.bitcast(F32R))

    def re(self, pat, **kw):
        return V(self.buf, self.ap.rearrange(pat, **kw))


class Op:
    __slots__ = ('eng', 'fn', 'kind', 'deps', 'signal', 'val', 'semslot', 'gidx', 'key')

    def __init__(self, eng, fn, kind):
        self.eng = eng
        self.fn = fn
        self.kind = kind
        self.deps = []
        self.signal = False
        self.val = None
        self.semslot = None
        self.gidx = 0
        self.key = 0


class Prog:
    def __init__(self, nc, stack, n_dma_sems=96):
        self.nc = nc
        self.eobj = {'pe': nc.tensor, 'act': nc.scalar, 'dve': nc.vector, 'pool': nc.gpsimd, 'sp': nc.sync}
        self.esem = {e: stack.enter_context(nc.semaphore('es_' + e)) for e in ENGS}
        self.ecount = {e: 0 for e in ENGS}
        self.slots = [[stack.enter_context(nc.semaphore('ds%d' % i)), 0] for i in range(n_dma_sems)]
        self.free_slots = list(range(n_dma_sems))
        self.ops = {e: [] for e in ENGS}
        self.waited = {e: {} for e in ENGS}
        self.phase_bufs = []
        self.pstack = None
        self.nphase = 0
        self.uid = 0
        self.gcount = 0

    def begin(self):
        self.pstack = ExitStack()
        self.pstack.__enter__()

    def _name(self, name):
        self.uid += 1
        return '%s_%d' % (name, self.uid)

    def tile(self, shape, dtype=F32, name='t', stack=None):
        st = stack or self.pstack
        h = st.enter_context(self.nc.sbuf_tensor(self._name(name), list(shape), dtype))
        b = Buf(name)
        self.phase_bufs.append(b)
        return V(b, h[:])

    def psum(self, shape=(128, 512), dtype=F32, name='ps'):
        h = self.pstack.enter_context(self.nc.psum_tensor(self._name(name), list(shape), dtype))
        b = Buf(name)
        self.phase_bufs.append(b)
        return V(b, h[:])

    def _record(self, op, reads, writes):
        deps = []
        for v in reads:
            b = v.buf
            if b.tracked and b.last_w is not None:
                deps.append(b.last_w)
        for v in writes:
            b = v.buf
            if b.tracked:
                if b.last_w is not None:
                    deps.append(b.last_w)
                deps.extend(b.readers)
        self.gcount += 1
        op.gidx = self.gcount
        for d in deps:
            if d is op:
                continue
            op.key = max(op.key, d.gidx if d.kind == 'c' else d.key)
            if d.kind == 'c':
                if d.eng == op.eng and d.eng == 'pe' and op.kind == 'c':
                    continue
                d.signal = True
            op.deps.append(d)
        for v in reads:
            if v.buf.tracked:
                v.buf.readers.append(op)
        for v in writes:
            if v.buf.tracked:
                v.buf.last_w = op
                v.buf.readers = []
        self.ops[op.eng].append(op)

    def op(self, eng, fn, reads=(), writes=()):
        o = Op(eng, fn, 'c')
        self._record(o, reads, writes)
        return o

    def dma(self, out, in_, q='sp', n=1, fn=None, **kw):
        sb = out.buf if out.buf.tracked else in_.buf
        assert sb.tracked
        if sb.semslot is None:
            sb.semslot = self.free_slots.pop()
        o = Op(q, None, 'd')
        o.semslot = sb.semslot
        slot = self.slots[sb.semslot]
        slot[1] += 16 * n
        o.val = slot[1]
        if fn is None:
            oap, iap = out.ap, in_.ap
            o.fn = lambda e: [e.dma_start(out=oap, in_=iap, **kw)]
        else:
            o.fn = fn
        self._record(o, [in_], [out])
        return o

    def dmar(self, out, in_, **kw):
        return self.dma(out.r(), in_.r(), **kw)

    def flush(self):
        nc = self.nc
        for e in ENGS:
            for o in self.ops[e]:
                if o.kind == 'c' and o.signal:
                    self.ecount[e] += 1
                    o.val = self.ecount[e]
        used_slots = set()
        for e in ENGS:
            for o in self.ops[e]:
                if o.kind == 'd':
                    used_slots.add(o.semslot)
        drain_eng = 'sp'
        with nc.Block() as block:
            secs = {'pe': block.tensor, 'act': block.scalar, 'dve': block.vector, 'pool': block.gpsimd,
                    'sp': block.sync}
            for e in ENGS:
                ops = self.ops[e]
                if not ops and e != drain_eng:
                    continue

                def body(eo, ops=ops, e=e):
                    waited = self.waited[e]
                    for o in ops:
                        for d in o.deps:
                            if d.kind == 'c':
                                key = ('e', d.eng)
                                sem = self.esem[d.eng]
                            else:
                                key = ('d', d.semslot)
                                sem = self.slots[d.semslot][0]
                            if waited.get(key, 0) >= d.val:
                                continue
                            eo.wait_ge(sem, d.val)
                            waited[key] = d.val
                        ins = o.fn(eo)
                        if o.kind == 'c':
                            if o.signal:
                                ins.then_inc(self.esem[e], 1)
                        else:
                            for i in ins:
                                i.then_inc(self.slots[o.semslot][0], 16)
                    if e == drain_eng:
                        for s in sorted(used_slots):
                            sem, cnt = self.slots[s]
                            if waited.get(('d', s), 0) < cnt:
                                eo.wait_ge(sem, cnt)
                                waited[('d', s)] = cnt

                secs[e](body)
        for b in self.phase_bufs:
            b.last_w = None
            b.readers = []
            if b.semslot is not None:
                self.free_slots.append(b.semslot)
                b.semslot = None
        self.phase_bufs = []
        self.ops = {e: [] for e in ENGS}
        self.pstack.__exit__(None, None, None)
        self.pstack = None
        self.nphase += 1

    def mm(self, out, lhsT, rhs, start, stop):
        o, l, r = out.ap, lhsT.ap, rhs.ap
        return self.op('pe', lambda e: e.matmul(o, l, r, start=start, stop=stop), [lhsT, rhs], [out])

    def transpose(self, out, in_, ident):
        o, i, d = out.ap, in_.ap, ident.ap
        return self.op('pe', lambda e: e.transpose(o, i, d), [in_, ident], [out])

    def act(self, out, in_, func, scale=1.0, bias=0.0, eng='act', extra_reads=()):
        o, i = out.ap, in_.ap
        sc = scale.ap if isinstance(scale, V) else scale
        bi = bias.ap if isinstance(bias, V) else bias
        rd = [in_] + [x for x in (scale, bias) if isinstance(x, V)] + list(extra_reads)
        return self.op('act', lambda e: e.activation(o, i, func, bias=bi, scale=sc), rd, [out])

    def ts(self, out, in0, s1, s2, op0, op1=None, eng='dve'):
        o, i = out.ap, in0.ap
        a1 = s1.ap if isinstance(s1, V) else s1
        a2 = s2.ap if isinstance(s2, V) else s2
        rd = [in0] + [x for x in (s1, s2) if isinstance(x, V)]
        if op1 is None:
            return self.op(eng, lambda e: e.tensor_scalar(o, i, a1, None, op0), rd, [out])
        return self.op(eng, lambda e: e.tensor_scalar(o, i, a1, a2, op0, op1), rd, [out])

    def tt(self, out, in0, in1, op, eng='dve'):
        o, a, b = out.ap, in0.ap, in1.ap
        return self.op(eng, lambda e: e.tensor_tensor(o, a, b, op), [in0, in1], [out])

    def stt(self, out, in0, scalar, in1, op0, op1):
        o, a, b = out.ap, in0.ap, in1.ap
        s = scalar.ap if isinstance(scalar, V) else scalar
        rd = [in0, in1] + ([scalar] if isinstance(scalar, V) else [])
        return self.op('dve', lambda e: e.scalar_tensor_tensor(o, a, s, b, op0, op1), rd, [out])

    def copy(self, out, in_, eng='dve'):
        o, i = out.ap, in_.ap
        if eng == 'act':
            return self.op('act', lambda e: e.copy(o, i), [in_], [out])
        return self.op(eng, lambda e: e.tensor_copy(o, i), [in_], [out])

    def recip(self, out, in_):
        o, i = out.ap, in_.ap
        return self.op('dve', lambda e: e.reciprocal(o, i), [in_], [out])

    def memset(self, out, val, eng='dve'):
        o = out.ap
        return self.op(eng, lambda e: e.memset(o, val), [], [out])


class Ring:
    def __init__(self, items):
        self.items = items
        self.i = 0

    def next(self):
        it = self.items[self.i % len(self.items)]
        self.i += 1
        return it


def dram(nc, name, shape, dtype=F32, kind='Internal'):
    h = nc.dram_tensor(name, list(shape), dtype, kind=kind)
    return V(Buf(name, tracked=False), h.ap())


class Net:
    def __init__(self, nc, stack, dbg=(), stop=None):
        self.nc = nc
        self.dbg = set(dbg)
        self.stop = stop
        self.p = Prog(nc, stack)
        self.stack = stack
        p = self.p
        self.in_shapes = {
            'xT': ([D, T], F32), 'c': ([1, D], F32), 'ctxT': ([D, TC], F32), 'c_ctx': ([1, D], F32),
            'ada_w': ([DEPTH, D, 6 * D], F32), 'ada_b': ([DEPTH, 6 * D], F32),
            'norm1_g': ([DEPTH, D], F32), 'norm2_g': ([DEPTH, D], F32), 'w_in': ([DEPTH, D, IN_W], F32),
            'biasx': ([DEPTH * NH, 20, 128, 512], F32), 'w_att_o': ([DEPTH, D, D], F32),
            'w_fourier': ([DEPTH, 512, D], F32), 'w_pool': ([DEPTH, 4, 128, 256], F32),
            'pool_scale': ([DEPTH, D], F32), 'w_out': ([DEPTH, D, D], F32), 'w_router': ([DEPTH, D, NE], F32),
            'w_g': ([DEPTH * NE, D, FF], F32), 'w_u': ([DEPTH * NE, D, FF], F32),
            'w_d': ([DEPTH * NE, FF, D], F32), 'final_g': ([1, D], F32),
            'dftc': ([T, T], BF16), 'dfts': ([T, T], BF16), 'dftc_c': ([TC, TC], BF16),
            'dfts_c': ([TC, TC], BF16), 'dft128': ([128, 256], F32), 'invcnt': ([4, T], F32),
            'invcnt_c': ([4, TC], F32), 'ident': ([128, 128], F32), 'iota512': ([128, 512], F32),
            'jcol': ([128, 4], F32)}
        self.ins = {}
        self.out = dram(nc, 'outT', [D, T], F32, 'ExternalOutput')
        self.scr = {}

    def __getattr__(self, name):
        d = self.__dict__
        if 'in_shapes' in d and name in d['in_shapes']:
            if name not in d['ins']:
                sh, dt = d['in_shapes'][name]
                d['ins'][name] = dram(d['nc'], name, sh, dt, 'ExternalInput')
            return d['ins'][name]
        raise AttributeError(name)

    def S(self, name, shape, dtype=F32):
        if name not in self.scr:
            kind = 'ExternalOutput' if name in self.dbg else 'Internal'
            self.scr[name] = dram(self.nc, name, shape, dtype, kind)
        return self.scr[name]

    def col_load(self, dst, src_row):
        p = self.p
        oap = dst.ap
        iap = src_row.ap.rearrange("o (k q) -> q (o k)", q=128)
        p.dma(dst, src_row, fn=lambda e: [e.dma_start(out=oap, in_=iap, allow_slow_non_contiguous=True)])

    def phase_adaln(self):
        p = self.p
        p.begin()
        mod = self.S('mod', [DEPTH * 2, 6 * D])
        craw = p.tile([128, 8, 2], F32, 'craw')
        s2 = p.tile([128, 8, 2], F32, 's2')
        self.col_load(craw[:, :, 0], self.c)
        self.col_load(craw[:, :, 1], self.c_ctx)
        p.act(s2, craw, AF.Silu)
        wb = Ring([p.tile([128, 8, 512], F32, 'adaw') for _ in range(6)])
        bb = Ring([p.tile([2, 512], F32, 'adab') for _ in range(6)])
        ob = Ring([p.tile([2, 512], F32, 'modo') for _ in range(2)])
        ps = Ring([p.psum([128, 512], F32, 'adaps') for _ in range(2)])
        lq = []

        def aload(i):
            l, nb = divmod(i, 12)
            w = wb.next()
            p.dma(w, self.ada_w[l, :, nb * 512:(nb + 1) * 512].re("(k q) n -> q k n", q=128))
            b = bb.next()
            p.dma(b, V(self.ada_b.buf, self.ada_b.ap[l:l + 1, nb * 512:(nb + 1) * 512].partition_broadcast(2)
                       .rearrange("p o n -> p (o n)")))
            lq.append((w, b))

        for i in range(4):
            aload(i)
        for l in range(DEPTH):
            for nb in range(12):
                i = l * 12 + nb
                if i + 4 < DEPTH * 12:
                    aload(i + 4)
                w, b = lq[i]
                acc = ps.next()
                for k in range(8):
                    p.mm(acc[0:2, :], s2[:, k, :], w[:, k, :], k == 0, k == 7)
                o = ob.next()
                p.tt(o, acc[0:2, :], b, ALU.add)
                p.dma(mod[2 * l:2 * l + 2, nb * 512:(nb + 1) * 512], o)
        p.flush()

    def load_modcols(self, l, si, seg, name):
        p = self.p
        mod = self.S('mod', [DEPTH * 2, 6 * D])
        t = p.tile([128, 8], F32, name)
        self.col_load(t, mod[2 * l + si:2 * l + si + 1, seg * D:(seg + 1) * D])
        return t

    def norm_consts(self, l, si, which):
        p = self.p
        base = 0 if which == 1 else 3
        sh = self.load_modcols(l, si, base + 0, 'sh')
        sc = self.load_modcols(l, si, base + 1, 'sc')
        g = p.tile([128, 8], F32, 'ng')
        ng = self.norm1_g if which == 1 else self.norm2_g
        self.col_load(g, ng[l:l + 1, :])
        gs = p.tile([128, 8], F32, 'gs')
        p.stt(gs, sc, 1.0, g, ALU.add, ALU.mult)
        return gs, sh

    def norm_load(self, xsrc, t0, nt, xring):
        xb = xring.next()
        self.p.dma(xb[:, :, 0:nt], xsrc[:, t0:t0 + nt].re("(k q) t -> q k t", q=128))
        return xb

    def emit_norm(self, xsrc, t0, nt, gs, sh, hT, hoff, ones, xring, sqring, tmpring, psring, rsring, xb=None):
        p = self.p
        if xb is None:
            xb = self.norm_load(xsrc, t0, nt, xring)
        acc = psring.next()
        for k in range(8):
            sq = sqring.next()
            p.act(sq[:, 0:nt], xb[:, k, 0:nt], AF.Square)
            p.mm(acc[:, 0:nt], ones, sq[:, 0:nt], k == 0, k == 7)
        rs = rsring.next()
        p.act(rs[:, 0:nt], acc[:, 0:nt], AF.Sqrt, scale=1.0 / D, bias=self.epsc)
        p.recip(rs[:, 0:nt], rs[:, 0:nt])
        for k in range(8):
            tmp = tmpring.next()
            p.tt(tmp[:, 0:nt], xb[:, k, 0:nt], rs[:, 0:nt], ALU.mult)
            p.act(hT[:, k, hoff:hoff + nt].r(), tmp[:, 0:nt], AF.Identity, scale=gs[:, k:k + 1], bias=sh[:, k:k + 1])

    def phase_normproj(self, l, xsrc, cxsrc, full_ctx):
        p = self.p
        dst = {}
        for si, Tn in ((0, T), (1, TC)):
            sfx = '%d_%d' % (l, si)
            d = {'k': self.S('kT' + sfx, [D, Tn], BF16), 'v': self.S('v' + sfx, [Tn, D], BF16)}
            if si == 0 or full_ctx:
                d['q'] = self.S('qT' + sfx, [D, Tn], BF16)
                d['u'] = self.S('uT' + sfx, [512, Tn])
                d['p'] = self.S('pT' + sfx, [512, Tn])
                d['g'] = self.S('gT' + sfx, [3 * D, Tn])
            dst[si] = d
        TBIG = 2048
        p.begin()
        cons = {0: self.norm_consts(l, 0, 1), 1: self.norm_consts(l, 1, 1)}
        srcs = {0: xsrc, 1: cxsrc}
        ones = p.tile([128, 128], F32, 'ones')
        p.memset(ones, 1.0)
        self.epsc = p.tile([128, 1], F32, 'epsc')
        p.memset(self.epsc, EPS)
        hT = p.tile([128, 8, TBIG + TC], F32, 'hT')
        xring = Ring([p.tile([128, 8, 512], F32, 'xb') for _ in range(2)])
        sqring = Ring([p.tile([128, 512], F32, 'sq') for _ in range(3)])
        tmpring = Ring([p.tile([128, 512], F32, 'tmp') for _ in range(3)])
        rsring = Ring([p.tile([128, 512], F32, 'rs') for _ in range(2)])
        psn = Ring([p.psum([128, 512], F32, 'psn') for _ in range(2)])
        psm = Ring([p.psum([128, 512], F32, 'psm') for _ in range(6)])
        wring = Ring([p.tile([128, 8, 512], F32, 'wblk') for _ in range(3)])
        o32 = Ring([p.tile([128, 512], F32, 'o32') for _ in range(8)])
        o16 = Ring([p.tile([128, 512], BF16, 'o16') for _ in range(10)])
        ev = 0
        passes = [[(0, 0, TBIG, 0), (1, 0, TC, TBIG)], [(0, TBIG, TBIG, 0)]]
        for segs in passes:
            for si, t0, n, ho in segs:
                nb = min(512, n)
                for bi in range(n // nb):
                    self.emit_norm(srcs[si], t0 + bi * nb, nb, cons[si][0], cons[si][1], hT, ho + bi * nb, ones,
                                   xring, sqring, tmpring, psn, rsring)
            wq = []

            def wload(cb):
                w = wring.next()
                p.dmar(w, self.w_in[l, :, cb * 512:(cb + 1) * 512].re("(k q) n -> q k n", q=128))
                wq.append(w)

            wload(0)
            wload(1)
            for cb in range(14):
                if cb + 2 < 14:
                    wload(cb + 2)
                w = wq[cb]
                c0 = cb * 512
                for si, t0, n, ho in segs:
                    d = dst[si]
                    if si == 1 and not full_ctx and c0 >= Q_OFF:
                        continue
                    nb = min(512, n)
                    if V_OFF <= c0 < Q_OFF:
                        for tt_ in range(n // 128):
                            acc = psm.next()
                            for k in range(8):
                                p.mm(acc, hT[:, k, ho + tt_ * 128:ho + (tt_ + 1) * 128].r(), w[:, k, :].r(),
                                     k == 0, k == 7)
                            o = o16.next()
                            ev += 1
                            p.copy(o, acc, eng='act' if ev % 2 else 'dve')
                            tok0 = t0 + tt_ * 128
                            p.dma(d['v'][tok0:tok0 + 128, c0 - V_OFF:c0 - V_OFF + 512], o)
                        continue
                    for ch in range(4):
                        col = c0 + ch * 128
                        for bi in range(n // nb):
                            acc = psm.next()
                            for k in range(8):
                                p.mm(acc[:, 0:nb], w[:, k, ch * 128:(ch + 1) * 128].r(),
                                     hT[:, k, ho + bi * nb:ho + (bi + 1) * nb].r(), k == 0, k == 7)
                            tok0 = t0 + bi * nb
                            ev += 1
                            if col < V_OFF:
                                o = o16.next()
                                p.copy(o[:, 0:nb], acc[:, 0:nb], eng='act' if ev % 2 else 'dve')
                                p.dma(d['k'][col:col + 128, tok0:tok0 + nb], o[:, 0:nb])
                            elif col < F_OFF:
                                o = o16.next()
                                if ev % 2:
                                    p.act(o[:, 0:nb], acc[:, 0:nb], AF.Copy, scale=HD ** -0.5)
                                else:
                                    p.ts(o[:, 0:nb], acc[:, 0:nb], HD ** -0.5, None, ALU.mult)
                                p.dma(d['q'][col - Q_OFF:col - Q_OFF + 128, tok0:tok0 + nb], o[:, 0:nb])
                            elif col < G_OFF:
                                o = o32.next()
                                p.copy(o[:, 0:nb], acc[:, 0:nb], eng='act' if ev % 2 else 'dve')
                                dd = d['u'] if col < P_OFF else d['p']
                                r0 = (col - F_OFF) % 512
                                p.dma(dd[r0:r0 + 128, tok0:tok0 + nb], o[:, 0:nb])
                            else:
                                o = o32.next()
                                p.act(o[:, 0:nb], acc[:, 0:nb], AF.Sigmoid)
                                p.dma(d['g'][col - G_OFF:col - G_OFF + 128, tok0:tok0 + nb], o[:, 0:nb])
        p.flush()

    def phase_attn(self, l, with_ctxq):
        p = self.p
        kT = self.S('kT%d_0' % l, [D, T], BF16)
        qT = self.S('qT%d_0' % l, [D, T], BF16)
        vv = self.S('v%d_0' % l, [T, D], BF16)
        kTc = self.S('kT%d_1' % l, [D, TC], BF16)
        vc = self.S('v%d_1' % l, [TC, D], BF16)
        attT = self.S('attT%d_0' % l, [D, T])
        if with_ctxq:
            qTc = self.S('qT%d_1' % l, [D, TC], BF16)
            attTc = self.S('attT%d_1' % l, [D, TC])
        p.begin()
        vall = p.tile([128, 32, D], BF16, 'vall')
        p.dma(vall, vv.re("(i q) d -> q i d", q=128))
        vcall = p.tile([128, 2, D], BF16, 'vcall')
        p.dma(vcall, vc.re("(i q) d -> q i d", q=128))
        idf = p.tile([128, 128], F32, 'idf')
        p.dma(idf, self.ident)
        idb = p.tile([128, 128], BF16, 'idb')
        p.copy(idb, idf)
        onesb = p.tile([128, 128], BF16, 'onesb2')
        p.memset(onesb, 1.0)
        khz = [p.tile([128, T], BF16, 'khz') for _ in range(2)]
        kcz = [p.tile([128, TC], BF16, 'kcz') for _ in range(2)]
        for par in range(2):
            z = slice(64, 128) if par == 0 else slice(0, 64)
            p.memset(khz[par][z, :], 0.0, eng='pool')
            p.memset(kcz[par][z, :], 0.0, eng='pool')
        qpr = Ring([p.tile([128, T], BF16, 'qp') for _ in range(2)])
        qcr = Ring([p.tile([128, TC], BF16, 'qcp') for _ in range(2)])
        bhr = Ring([p.tile([128, 20, 512], BF16, 'bh') for _ in range(2)])
        psS = Ring([p.psum([128, 512], F32, 'psS') for _ in range(4)])
        psN = Ring([p.psum([128, 512], F32, 'psN') for _ in range(2)])
        psD = Ring([p.psum([128, 512], F32, 'psD') for _ in range(2)])
        pr = Ring([p.tile([128, 512], BF16, 'pt') for _ in range(6)])
        rcr = Ring([p.tile([128, 512], F32, 'rc') for _ in range(2)])
        aor = Ring([p.tile([128, 512], F32, 'ao') for _ in range(3)])
        hq = {}

        def hload(h):
            hs = slice(h * 64, (h + 1) * 64)
            par = h % 2
            hp = slice(par * 64, par * 64 + 64)
            ps_ = slice((h // 2) * 128, (h // 2 + 1) * 128)
            p.dma(khz[par][hp, :], kT[hs, :])
            p.dma(kcz[par][hp, :], kTc[hs, :])
            if par == 0:
                qh = qpr.next()
                p.dma(qh, qT[ps_, :])
                qch = None
                if with_ctxq:
                    qch = qcr.next()
                    p.dma(qch, qTc[ps_, :])
                hq[h // 2] = (qh, qch)
            bh = bhr.next()
            p.dma(bh, self.biasx[l * NH + h].re("n q f -> q n f"), q='pool')
            hq[('b', h)] = bh

        hload(0)
        for h in range(NH):
            hs = slice(h * 64, (h + 1) * 64)
            par = h % 2
            hp = slice(par * 64, par * 64 + 64)
            ps_ = slice((h // 2) * 128, (h // 2 + 1) * 128)
            if h + 1 < NH:
                hload(h + 1)
            kh = khz[par]
            kch = kcz[par]
            qh, qch = hq[h // 2]
            bh = hq[('b', h)]
            jobs = [(j, 512) for j in range(8)] + ([(-1, TC)] if with_ctxq else [])
            for j, nq in jobs:
                if j >= 0:
                    tiles, base = chunk_tiles(j)
                    qv = qh[:, j * 512:(j + 1) * 512]
                else:
                    tiles, base = [], 0
                    qv = qch[:, 0:TC]
                num = psN.next()
                den = psD.next()
                ntot = len(tiles) + 2
                pend = []

                def pv(item, num=num, den=den, nq=nq, ntot=ntot):
                    ti, vt, pt = item
                    p.mm(num[:, 0:nq], vt, pt[:, 0:nq], ti == 0, ti == ntot - 1)
                    p.mm(den[:, 0:nq], onesb, pt[:, 0:nq], ti == 0, ti == ntot - 1)

                for ti in range(ntot):
                    S_ = psS.next()
                    if ti < len(tiles):
                        m = tiles[ti]
                        p.mm(S_[:, 0:nq], kh[:, m * 128:(m + 1) * 128], qv, True, False)
                        p.mm(S_[:, 0:nq], idb, bh[:, base + ti, :], False, True)
                        vt = vall[:, m, ps_]
                    else:
                        cm = ti - len(tiles)
                        p.mm(S_[:, 0:nq], kch[:, cm * 128:(cm + 1) * 128], qv, True, True)
                        vt = vcall[:, cm, ps_]
                    pt = pr.next()
                    p.act(pt[:, 0:nq], S_[:, 0:nq], AF.Exp)
                    pend.append((ti, vt, pt))
                    if len(pend) > 2:
                        pv(pend.pop(0))
                while pend:
                    pv(pend.pop(0))
                rc = rcr.next()
                p.recip(rc[hp, 0:nq], den[hp, 0:nq])
                ao = aor.next()
                p.tt(ao[hp, 0:nq], num[hp, 0:nq], rc[hp, 0:nq], ALU.mult)
                if j >= 0:
                    p.dma(attT[hs, j * 512:(j + 1) * 512], ao[hp, :])
                else:
                    p.dma(attTc[hs, 0:TC], ao[hp, 0:TC])
        p.flush()

    def phase_fourier_pool(self, l, si):
        p = self.p
        Tn = T if si == 0 else TC
        NI = Tn // 128
        NBk = min(1024, Tn)
        nh = (NBk + 511) // 512
        hw = min(512, NBk)
        sfx = '%d_%d' % (l, si)
        uT = self.S('uT' + sfx, [512, Tn])
        pT = self.S('pT' + sfx, [512, Tn])
        fourT = self.S('fourT' + sfx, [512, Tn])
        poolT = self.S('poolT' + sfx, [512, Tn])
        tabs = (self.dftc, self.dfts) if si == 0 else (self.dftc_c, self.dfts_c)
        icn = self.invcnt if si == 0 else self.invcnt_c
        p.begin()
        d128 = p.tile([128, 256], F32, 'd128')
        p.dmar(d128, self.dft128)
        AB = p.tile([128, NI, 4, 256], BF16, 'AB')
        ub = p.tile([128, Tn], F32, 'ub')
        acc = [[p.psum([128, 512], F32, 'facc') for _ in range(nh)] for _ in range(4)]
        ps1 = Ring([acc[0][0], acc[1][0]])
        pring = Ring([p.tile([128, 8, NBk], BF16, 'dpc') for _ in range(3)])
        o32 = Ring([p.tile([128, 512], F32, 'fo') for _ in range(4)])
        L = Tn + 32
        U = p.tile([128, L], F32, 'pU')
        A = p.tile([128, L], F32, 'pA')
        B = p.tile([128, L], F32, 'pB')
        iclo = p.tile([128, 16], F32, 'iclo')
        ichi = p.tile([128, 16], F32, 'ichi')
        for g in range(4):
            w = 2 << g
            half = w // 2
            eng = 'pool'
            p.memset(U[:, 0:16], 0.0, eng=eng)
            p.memset(U[:, 16 + Tn:L], 0.0, eng=eng)
            p.dma(U[:, 16:16 + Tn], pT[g * 128:(g + 1) * 128, :], q='pool')
            p.dma(iclo, V(icn.buf, icn.ap[g:g + 1, 0:16].partition_broadcast(128).rearrange("p o n -> p (o n)")), q='pool')
            p.dma(ichi, V(icn.buf, icn.ap[g:g + 1, Tn - 16:Tn].partition_broadcast(128)
                          .rearrange("p o n -> p (o n)")), q='pool')
            cur, n, bufs, bi = U, 1, [A, B], 0
            while n < w:
                nxt = bufs[bi]
                bi ^= 1
                ln = L - (2 * n - 1)
                p.tt(nxt[:, 0:ln], cur[:, 0:ln], cur[:, n:n + ln], ALU.add, eng=eng)
                cur = nxt
                n *= 2
            res = bufs[bi]
            s0 = 16 - half
            p.ts(res[:, 0:Tn], cur[:, s0:s0 + Tn], 1.0 / w, None, ALU.mult, eng=eng)
            p.tt(res[:, 0:16], cur[:, s0:s0 + 16], iclo, ALU.mult, eng=eng)
            p.tt(res[:, Tn - 16:Tn], cur[:, s0 + Tn - 16:s0 + Tn], ichi, ALU.mult, eng=eng)
            p.tt(res[:, 0:Tn], res[:, 0:Tn], U[:, 16:16 + Tn], ALU.subtract, eng=eng)
            p.dma(poolT[g * 128:(g + 1) * 128, :], res[:, 0:Tn], q='pool')
        ev = 0
        for g in range(4):
            p.dmar(ub, uT[g * 128:(g + 1) * 128, :])
            for i in range(NI):
                ps = ps1.next()
                p.mm(ps[:, 0:256], ub[:, i * 128:(i + 1) * 128].r(), d128.r(), True, True)
                p.copy(AB[:, i, g, 0:128], ps[:, 0:128], eng='act')
                p.ts(AB[:, i, g, 128:256], ps[:, 128:256], -1.0, None, ALU.mult)
        for kb in range(Tn // NBk):
            for ti, tbl in enumerate(tabs):
                for lg in range(max(1, NI // 8)):
                    nl = min(8, NI)
                    pc = pring.next()
                    p.dma(pc[:, 0:nl, :], tbl[lg * 1024:lg * 1024 + nl * 128, kb * NBk:(kb + 1) * NBk]
                          .re("(i q) k -> q i k", q=128))
                    for ii in range(nl):
                        i = lg * 8 + ii
                        for g in range(4):
                            for h in range(nh):
                                p.mm(acc[g][h][:, 0:hw], AB[:, i, g, ti * 128:(ti + 1) * 128],
                                     pc[:, ii, h * 512:h * 512 + hw], ti == 0 and i == 0, ti == 1 and i == NI - 1)
            for g in range(4):
                for h in range(nh):
                    o = o32.next()
                    ev += 1
                    p.copy(o[:, 0:hw], acc[g][h][:, 0:hw], eng='act' if ev % 2 else 'dve')
                    c0 = kb * NBk + h * 512
                    p.dma(fourT[g * 128:(g + 1) * 128, c0:c0 + hw], o[:, 0:hw])
        p.flush()

    def phase_merge(self, l, si, xsrc, xdst):
        p = self.p
        Tn = T if si == 0 else TC
        NB = min(512, Tn)
        sfx = '%d_%d' % (l, si)
        attT = self.S('attT' + sfx, [D, Tn])
        fourT = self.S('fourT' + sfx, [512, Tn])
        poolT = self.S('poolT' + sfx, [512, Tn])
        gT = self.S('gT' + sfx, [3 * D, Tn])
        p.begin()
        wa = p.tile([128, 8, D], F32, 'wa')
        p.dmar(wa, self.w_att_o[l].re("(k q) n -> q k n", q=128))
        wf = p.tile([128, 4, D], F32, 'wf')
        p.dmar(wf, self.w_fourier[l].re("(k q) n -> q k n", q=128))
        wp = p.tile([128, 4, 256], F32, 'wp')
        p.dmar(wp, self.w_pool[l].re("g c f -> c g f"))
        wo = p.tile([128, 8, D], F32, 'wo')
        p.dmar(wo, self.w_out[l].re("(k q) n -> q k n", q=128))
        g1 = self.load_modcols(l, si, 2, 'g1')
        psc = p.tile([128, 8], F32, 'psc')
        self.col_load(psc, self.pool_scale[l:l + 1, :])
        at = p.tile([128, 8, NB], F32, 'at')
        fo = p.tile([128, 4, NB], F32, 'fo')
        po = p.tile([128, 4, NB], F32, 'po')
        xbr = Ring([p.tile([128, 8, NB], F32, 'xb') for _ in range(2)])
        mg = p.tile([128, 8, NB], F32, 'mg')
        gring = Ring([p.tile([128, 3, NB], F32, 'gt') for _ in range(2)])
        tmp = Ring([p.tile([128, NB], F32, 'mt') for _ in range(6)])
        o32 = Ring([p.tile([128, NB], F32, 'xo') for _ in range(2)])
        psa = Ring([p.psum([128, 512], F32, 'psa') for _ in range(2)])
        psf = Ring([p.psum([128, 512], F32, 'psf') for _ in range(2)])
        psp = Ring([p.psum([128, 512], F32, 'psp') for _ in range(2)])
        pso = Ring([p.psum([128, 512], F32, 'pso') for _ in range(2)])
        gview = gT.re("(b k q) t -> q b k t", b=3, q=128)
        def load_in(tb):
            t0 = tb * NB
            p.dmar(at, attT[:, t0:t0 + NB].re("(k q) t -> q k t", q=128))
            p.dmar(fo, fourT[:, t0:t0 + NB].re("(k q) t -> q k t", q=128))
            p.dmar(po, poolT[:, t0:t0 + NB].re("(k q) t -> q k t", q=128))

        load_in(0)
        for tb in range(Tn // NB):
            t0 = tb * NB
            xb = xbr.next()
            p.dma(xb, xsrc[:, t0:t0 + NB].re("(k q) t -> q k t", q=128))
            for dc in range(8):
                cs = slice(dc * 128, (dc + 1) * 128)
                gt = gring.next()
                p.dma(gt, gview[:, :, dc, t0:t0 + NB])
                ya = psa.next()
                for k in range(8):
                    p.mm(ya[:, 0:NB], wa[:, k, cs].r(), at[:, k, :].r(), k == 0, k == 7)
                yf = psf.next()
                for k in range(4):
                    p.mm(yf[:, 0:NB], wf[:, k, cs].r(), fo[:, k, :].r(), k == 0, k == 3)
                yp = psp.next()
                p.mm(yp[:, 0:NB], wp[:, dc // 2, (dc % 2) * 128:(dc % 2 + 1) * 128].r(), po[:, dc // 2, :].r(),
                     True, True)
                m1, m2, m3 = tmp.next(), tmp.next(), tmp.next()
                p.tt(m1, ya[:, 0:NB], gt[:, 0, :], ALU.mult)
                p.tt(m2, yf[:, 0:NB], gt[:, 1, :], ALU.mult)
                p.stt(m3, yp[:, 0:NB], psc[:, dc:dc + 1], gt[:, 2, :], ALU.mult, ALU.mult)
                p.tt(m1, m1, m2, ALU.add, eng='pool')
                p.tt(mg[:, dc, :].r(), m1, m3, ALU.add)
            if tb + 1 < Tn // NB:
                load_in(tb + 1)
            for dc in range(8):
                cs = slice(dc * 128, (dc + 1) * 128)
                yo = pso.next()
                for k in range(8):
                    p.mm(yo[:, 0:NB], wo[:, k, cs].r(), mg[:, k, :].r(), k == 0, k == 7)
                xo = o32.next()
                p.stt(xo, yo[:, 0:NB], g1[:, dc:dc + 1], xb[:, dc, :], ALU.mult, ALU.add)
                p.dma(xdst[cs, t0:t0 + NB], xo)
        p.flush()

    def phase_route(self, l, si, xsrc):
        p = self.p
        Tn = T if si == 0 else TC
        NI = Tn // 128
        NB = min(512, Tn)
        cap = 2 * Tn // NE
        sfx = '%d_%d' % (l, si)
        h2 = self.S('h2tok' + sfx, [Tn, D], BF16)
        posT = self.S('posT' + sfx, [NE, Tn])
        affTd = self.S('affT' + sfx, [NE, Tn])
        postm = self.S('postm' + sfx, [128, NI * NE])
        p.begin()
        gs, sh = self.norm_consts(l, si, 2)
        ones = p.tile([128, 128], F32, 'ones')
        p.memset(ones, 1.0)
        self.epsc = p.tile([128, 1], F32, 'epsc')
        p.memset(self.epsc, EPS)
        idf = p.tile([128, 128], F32, 'idf')
        p.dma(idf, self.ident)
        wr = p.tile([128, 8, NE], F32, 'wr')
        p.dma(wr, self.w_router[l].re("(k q) e -> q k e", q=128))
        eT = p.tile([NE, Tn], F32, 'eT')
        affT = p.tile([NE, Tn], F32, 'affT')
        hring = Ring([p.tile([128, 8, NB], F32, 'h2T') for _ in range(2)])
        xring = Ring([p.tile([128, 8, NB], F32, 'xb') for _ in range(2)])
        sqring = Ring([p.tile([128, NB], F32, 'sq') for _ in range(3)])
        tmpring = Ring([p.tile([128, NB], F32, 'tmp') for _ in range(3)])
        rsring = Ring([p.tile([128, NB], F32, 'rs') for _ in range(2)])
        htr = Ring([p.tile([128, D], BF16, 'htok') for _ in range(3)])
        psn = Ring([p.psum([128, 512], F32, 'psn') for _ in range(2)])
        psl = Ring([p.psum([128, 512], F32, 'psl') for _ in range(2)])
        pst = Ring([p.psum([128, 512], F32, 'pst') for _ in range(4)])
        ev = 0
        xq = {0: self.norm_load(xsrc, 0, NB, xring)}
        for bi in range(Tn // NB):
            hT = hring.next()
            if bi + 1 < Tn // NB:
                xq[bi + 1] = self.norm_load(xsrc, (bi + 1) * NB, NB, xring)
            self.emit_norm(xsrc, bi * NB, NB, gs, sh, hT, 0, ones, xring, sqring, tmpring, psn, rsring, xb=xq[bi])
            lg = psl.next()
            for k in range(8):
                p.mm(lg[0:NE, 0:NB], wr[:, k, :], hT[:, k, :], k == 0, k == 7)
            p.act(eT[:, bi * NB:(bi + 1) * NB], lg[0:NE, 0:NB], AF.Exp)
            for tt_ in range(NB // 128):
                ht = htr.next()
                for hf in range(2):
                    pt = pst.next()
                    for kk in range(4):
                        k = hf * 4 + kk
                        p.transpose(pt[:, kk * 128:(kk + 1) * 128], hT[:, k, tt_ * 128:(tt_ + 1) * 128], idf)
                    ev += 1
                    p.copy(ht[:, hf * 512:(hf + 1) * 512], pt, eng='act' if ev % 2 else 'dve')
                tok0 = bi * NB + tt_ * 128
                p.dma(h2[tok0:tok0 + 128, :], ht)
        rcs = Ring([p.tile([NE, NB], F32, 'rcs') for _ in range(2)])
        for bi in range(Tn // NB):
            bs = slice(bi * NB, (bi + 1) * NB)
            sm = psl.next()
            p.mm(sm[0:NE, 0:NB], ones[0:NE, 0:NE], eT[:, bs], True, True)
            rc = rcs.next()
            p.recip(rc, sm[0:NE, 0:NB])
            p.tt(affT[:, bs], eT[:, bs], rc, ALU.mult)
        lo = p.tile([NE, 1], F32, 'lo')
        hi = p.tile([NE, 1], F32, 'hi')
        mid = p.tile([NE, 1], F32, 'mid')
        cnt = p.tile([NE, 1], F32, 'cnt')
        ge = p.tile([NE, 1], F32, 'ge')
        d1 = p.tile([NE, 1], F32, 'd1')
        junk = p.tile([NE, Tn], F32, 'junk')
        p.memset(lo, 0.0)
        p.memset(hi, 1.0)
        for it in range(30):
            p.ts(mid, lo, hi, 0.5, ALU.add, ALU.mult)
            ja, aa, ma, ca = junk.ap, affT.ap, mid.ap, cnt.ap
            p.op('dve', lambda e, ja=ja, aa=aa, ma=ma, ca=ca: e.tensor_scalar(ja, aa, ma, None, ALU.is_ge, ALU.add,
                                                                               accum_out=ca),
                 [affT, mid], [junk, cnt])
            p.ts(ge, cnt, float(cap), None, ALU.is_ge)
            p.tt(d1, mid, lo, ALU.subtract)
            p.stt(lo, d1, ge, lo, ALU.mult, ALU.add)
            p.tt(d1, hi, mid, ALU.subtract)
            p.stt(hi, d1, ge, mid, ALU.mult, ALU.add)
        mask = p.tile([NE, Tn], F32, 'mask')
        p.ts(mask, affT, lo, None, ALU.is_ge)
        p.memset(junk, 1.0)
        cum = p.tile([NE, Tn], F32, 'cum')
        ca, ja, ma = cum.ap, junk.ap, mask.ap
        p.op('dve', lambda e: e.tensor_tensor_scan(ca, ja, ma, 0.0, ALU.mult, ALU.add), [junk, mask], [cum])
        p.tt(cum, cum, mask, ALU.mult)
        p.ts(cum, cum, -1.0, None, ALU.add)
        p.dma(posT, cum)
        p.dma(affTd, affT)
        ptm = p.tile([128, NI * NE], F32, 'ptm')
        pt = pst.next()
        for i in range(NI):
            p.transpose(pt[:, i * NE:(i + 1) * NE], cum[:, i * 128:(i + 1) * 128], idf[0:NE, 0:NE])
        p.copy(ptm, pt[:, 0:NI * NE])
        p.dma(postm, ptm)
        p.flush()

    def phase_gather(self, l, streams):
        p = self.p
        p.begin()
        iota = p.tile([128, 512], F32, 'iota')
        p.dma(iota, self.iota512)
        acc = [p.psum([128, 512], F32, 'gacc') for _ in range(8)]
        sring = Ring([p.tile([128, 512], BF16, 'S') for _ in range(4)])
        xor_ = Ring([p.tile([128, 8, 512], F32, 'xgo') for _ in range(2)])
        ev = 0
        for si in streams:
            Tn = T if si == 0 else TC
            NI = Tn // 128
            cap = 2 * Tn // NE
            sfx = '%d_%d' % (l, si)
            h2 = self.S('h2tok' + sfx, [Tn, D], BF16)
            postm = self.S('postm' + sfx, [128, NI * NE])
            xg = self.S('xg' + sfx, [NE, D, cap])
            h2tok = p.tile([128, NI, D], BF16, 'h2tok')
            p.dma(h2tok, h2.re("(i q) d -> q i d", q=128))
            ptm = p.tile([128, NI, NE], F32, 'ptm')
            p.dma(ptm, postm.re("q (i e) -> q i e", e=NE))
            for e in range(NE):
                for i in range(NI):
                    S_ = sring.next()
                    p.ts(S_[:, 0:cap], iota[:, 0:cap], ptm[:, i, e:e + 1], None, ALU.is_equal)
                    for k in range(8):
                        p.mm(acc[k][:, 0:cap], h2tok[:, i, k * 128:(k + 1) * 128], S_[:, 0:cap], i == 0, i == NI - 1)
                xo = xor_.next()
                for k in range(8):
                    ev += 1
                    p.copy(xo[:, k, 0:cap], acc[k][:, 0:cap], eng='act' if ev % 2 else 'dve')
                p.dma(xg[e].re("(k q) c -> q k c", q=128), xo[:, :, 0:cap])
        p.flush()

    def phase_experts(self, l, streams):
        p = self.p
        p.begin()
        info = {}
        for si in streams:
            Tn = T if si == 0 else TC
            cap = 2 * Tn // NE
            Mp = cap if cap >= 128 else 128
            sfx = '%d_%d' % (l, si)
            xg = self.S('xg' + sfx, [NE, D, cap])
            ysc = self.S('y' + sfx, [NE, Mp, D], BF16)
            hT = p.tile([128, FF // 128, Mp], F32, 'ehT')
            if Mp != cap:
                p.memset(hT, 0.0)
            xr = Ring([p.tile([128, 8, cap], F32, 'exg') for _ in range(2)])
            yb = [p.psum([128, 512], F32, 'ey') for _ in range(Mp // 128)]
            info[si] = (cap, Mp, xg, ysc, hT, xr, yb)
        wgr = Ring([p.tile([128, 8, 512], F32, 'wg') for _ in range(2)])
        wur = Ring([p.tile([128, 8, 512], F32, 'wu') for _ in range(2)])
        wdr = Ring([p.tile([128, 4, 512], F32, 'wd') for _ in range(3)])
        npa = 8 - sum(len(info[si][6]) for si in streams)
        psa = Ring([p.psum([128, 512], F32, 'ea') for _ in range(max(1, npa - npa // 2))])
        psu = Ring([p.psum([128, 512], F32, 'eu') for _ in range(max(1, npa // 2))])
        slr = Ring([p.tile([128, 512], F32, 'sl') for _ in range(2)])
        yor = Ring([p.tile([128, 512], BF16, 'yo') for _ in range(5)])
        NFB = (FF + 511) // 512
        ev = 0
        xsq, guq, dq = {}, {}, {}

        def xs_load(e):
            if e >= NE or e in xsq:
                return
            xs = {}
            for si in streams:
                cap, Mp, xg, ysc, hT, xr, yb = info[si]
                xs[si] = xr.next()
                p.dmar(xs[si], xg[e].re("(k q) c -> q k c", q=128))
            xsq[e] = xs

        def gu_load(e, fb):
            if e >= NE or fb >= NFB or (e, fb) in guq:
                return
            ge = l * NE + e
            f0 = fb * 512
            fw = min(512, FF - f0)
            wgb = wgr.next()
            p.dmar(wgb[:, :, 0:fw], self.w_g[ge, :, f0:f0 + fw].re("(k q) f -> q k f", q=128))
            wub = wur.next()
            p.dmar(wub[:, :, 0:fw], self.w_u[ge, :, f0:f0 + fw].re("(k q) f -> q k f", q=128))
            guq[(e, fb)] = (wgb, wub)

        def d_load(e, dh, fb):
            if e >= NE or fb >= NFB or (e, dh, fb) in dq:
                return
            ge = l * NE + e
            f0 = fb * 512
            fw = min(512, FF - f0)
            wdb = wdr.next()
            p.dmar(wdb[:, 0:fw // 128, :], self.w_d[ge, f0:f0 + fw, dh * 512:(dh + 1) * 512]
                   .re("(c q) d -> q c d", q=128))
            dq[(e, dh, fb)] = wdb

        xs_load(0)
        gu_load(0, 0)
        for e in range(NE):
            xs = xsq[e]
            for fb in range(NFB):
                fw = min(512, FF - fb * 512)
                gu_load(e, fb)
                gu_load(e, fb + 1)
                wgb, wub = guq[(e, fb)]
                for fcl in range(fw // 128):
                    fc = fb * 4 + fcl
                    fs = slice(fcl * 128, (fcl + 1) * 128)
                    for si in streams:
                        cap, Mp, xg, ysc, hT, xr, yb = info[si]
                        a = psa.next()
                        for k in range(8):
                            p.mm(a[:, 0:cap], wgb[:, k, fs].r(), xs[si][:, k, :].r(), k == 0, k == 7)
                        u = psu.next()
                        for k in range(8):
                            p.mm(u[:, 0:cap], wub[:, k, fs].r(), xs[si][:, k, :].r(), k == 0, k == 7)
                        sl = slr.next()
                        p.act(sl[:, 0:cap], a[:, 0:cap], AF.Silu)
                        p.tt(hT[:, fc, 0:cap].r(), u[:, 0:cap], sl[:, 0:cap], ALU.mult)
                if fb == NFB - 2:
                    d_load(e, 0, 0)
            for dh in range(2):
                for fb in range(NFB):
                    fw = min(512, FF - fb * 512)
                    d_load(e, dh, fb)
                    d_load(e, dh, fb + 1)
                    wdb = dq[(e, dh, fb)]
                    for fcl in range(fw // 128):
                        fc = fb * 4 + fcl
                        for si in streams:
                            cap, Mp, xg, ysc, hT, xr, yb = info[si]
                            for sc in range(Mp // 128):
                                p.mm(yb[sc], hT[:, fc, sc * 128:(sc + 1) * 128].r(), wdb[:, fcl, :].r(),
                                     fc == 0, fc == FF // 128 - 1)
                if dh == 0:
                    d_load(e, 1, 0)
                    d_load(e, 1, 1)
                else:
                    xs_load(e + 1)
                    gu_load(e + 1, 0)
                    gu_load(e + 1, 1)
                for si in streams:
                    cap, Mp, xg, ysc, hT, xr, yb = info[si]
                    for sc in range(Mp // 128):
                        o = yor.next()
                        ev += 1
                        p.copy(o, yb[sc], eng='act' if ev % 2 else 'dve')
                        p.dma(ysc[e, sc * 128:(sc + 1) * 128, dh * 512:(dh + 1) * 512], o)
        p.flush()

    def phase_combine(self, l, si, xsrc, xdst):
        p = self.p
        Tn = T if si == 0 else TC
        NB = min(512, Tn)
        cap = 2 * Tn // NE
        Mp = cap if cap >= 128 else 128
        JC = Mp // 128
        sfx = '%d_%d' % (l, si)
        ysc = self.S('y' + sfx, [NE, Mp, D], BF16)
        posT = self.S('posT' + sfx, [NE, Tn])
        affTd = self.S('affT' + sfx, [NE, Tn])
        p.begin()
        g2 = self.load_modcols(l, si, 5, 'g2')
        jcol = p.tile([128, 4], F32, 'jcol')
        p.dma(jcol, self.jcol)
        yh = [p.tile([128, JC, 512], BF16, 'yh') for _ in range(NE)]
        posr = [p.tile([128, 8, NB], F32, 'posb') for _ in range(2)]
        affr = [p.tile([128, 8, NB], F32, 'affb') for _ in range(2)]
        xbr = Ring([p.tile([128, 4, NB], F32, 'xb') for _ in range(2)])
        sgr = Ring([p.tile([128, NB], BF16, 'sg') for _ in range(4)])
        o32 = Ring([p.tile([128, NB], F32, 'xo') for _ in range(4)])
        accs = Ring([[p.psum([128, 512], F32, 'cacc') for _ in range(4)] for _ in range(2)])
        yv = ysc.re("e (j q) d -> q e j d", q=128)
        ntb = Tn // NB
        steps = [(dh, tb) for dh in range(2) for tb in range(ntb)]

        def load_pa(step, eh):
            dh, tb = steps[step]
            t0 = tb * NB
            es = slice(eh * 8, (eh + 1) * 8)
            p.dma(posr[eh], V(posT.buf, posT.ap[es, t0:t0 + NB].partition_broadcast(128)))
            p.dma(affr[eh], V(affTd.buf, affTd.ap[es, t0:t0 + NB].partition_broadcast(128)))

        load_pa(0, 0)
        load_pa(0, 1)
        for st, (dh, tb) in enumerate(steps):
            t0 = tb * NB
            if tb == 0:
                for e in range(NE):
                    p.dma(yh[e], yv[:, e, :, dh * 512:(dh + 1) * 512])
            xb = xbr.next()
            p.dma(xb, xsrc[dh * 512:(dh + 1) * 512, t0:t0 + NB].re("(k q) t -> q k t", q=128))
            acc = accs.next()
            n = NE * JC
            idx = 0
            for eh in range(2):
                for e8 in range(8):
                    e = eh * 8 + e8
                    for jc in range(JC):
                        sg = sgr.next()
                        p.stt(sg, posr[eh][:, e8, :], jcol[:, jc:jc + 1], affr[eh][:, e8, :], ALU.is_equal,
                              ALU.mult)
                        for dc in range(4):
                            p.mm(acc[dc][:, 0:NB], yh[e][:, jc, dc * 128:(dc + 1) * 128], sg, idx == 0, idx == n - 1)
                        idx += 1
                if st + 1 < len(steps):
                    load_pa(st + 1, eh)
            for dc in range(4):
                c = dh * 4 + dc
                xo = o32.next()
                p.stt(xo, acc[dc][:, 0:NB], g2[:, c:c + 1], xb[:, dc, :], ALU.mult, ALU.add)
                p.dma(xdst[c * 128:(c + 1) * 128, t0:t0 + NB], xo)
        p.flush()

    def phase_final(self, xsrc):
        p = self.p
        p.begin()
        gs = p.tile([128, 8], F32, 'fg')
        self.col_load(gs, self.final_g)
        sh = p.tile([128, 8], F32, 'fsh')
        p.memset(sh, 0.0)
        ones = p.tile([128, 128], F32, 'ones')
        p.memset(ones, 1.0)
        self.epsc = p.tile([128, 1], F32, 'epsc')
        p.memset(self.epsc, EPS)
        NB = 512
        hring = Ring([p.tile([128, 8, NB], F32, 'oT') for _ in range(2)])
        xring = Ring([p.tile([128, 8, NB], F32, 'xb') for _ in range(2)])
        sqring = Ring([p.tile([128, NB], F32, 'sq') for _ in range(3)])
        tmpring = Ring([p.tile([128, NB], F32, 'tmp') for _ in range(3)])
        rsring = Ring([p.tile([128, NB], F32, 'rs') for _ in range(2)])
        psn = Ring([p.psum([128, 512], F32, 'psn') for _ in range(2)])
        xq = {0: self.norm_load(xsrc, 0, NB, xring)}
        for bi in range(T // NB):
            hT = hring.next()
            if bi + 1 < T // NB:
                xq[bi + 1] = self.norm_load(xsrc, (bi + 1) * NB, NB, xring)
            self.emit_norm(xsrc, bi * NB, NB, gs, sh, hT, 0, ones, xring, sqring, tmpring, psn, rsring, xb=xq[bi])
            p.dma(self.out[:, bi * NB:(bi + 1) * NB].re("(k q) t -> q k t", q=128), hT)
        p.flush()

    def build(self):
        self.phase_adaln()
        if self.stop == 'adaln':
            return
        x, cx = self.xT, self.ctxT
        for l in range(DEPTH):
            fc = l < DEPTH - 1
            st = lambda s: self.stop == '%s%d' % (s, l)
            self.phase_normproj(l, x, cx, fc)
            if st('proj'):
                return
            self.phase_attn(l, fc)
            if st('attn'):
                return
            self.phase_fourier_pool(l, 0)
            if fc:
                self.phase_fourier_pool(l, 1)
            if st('four'):
                return
            xm = self.S('xm%d' % l, [D, T])
            cm = self.S('cm%d' % l, [D, TC])
            self.phase_merge(l, 0, x, xm)
            if fc:
                self.phase_merge(l, 1, cx, cm)
            if st('merge'):
                return
            self.phase_route(l, 0, xm)
            if fc:
                self.phase_route(l, 1, cm)
            if st('route'):
                return
            streams = [0, 1] if fc else [0]
            self.phase_gather(l, streams)
            if st('gather'):
                return
            self.phase_experts(l, streams)
            if st('experts'):
                return
            xe = self.S('xe%d' % l, [D, T])
            ce = self.S('ce%d' % l, [D, TC])
            self.phase_combine(l, 0, xm, xe)
            if fc:
                self.phase_combine(l, 1, cm, ce)
            if st('combine'):
                return
            x, cx = xe, ce
        self.phase_final(x)


def build_nc(dbg=(), stop=None):
    nc = bass.Bass("TRN2", target_bir_lowering=False)
    nc.dge_precook = False
    stack = ExitStack()
    with stack:
        net = Net(nc, stack, dbg=dbg, stop=stop)
        net.build()
    return nc, net


def chunk_tiles(j):
    if j == 0:
        return list(range(0, 6)), 0
    if j == 7:
        return list(range(26, 32)), 14
    return list(range(4 * j - 2, 4 * j + 6)), 6


_CONSTS = None


def make_consts():
    global _CONSTS
    if _CONSTS is not None:
        return _CONSTS
    import ml_dtypes
    out = {}
    for name, L in (('', T), ('_c', TC)):
        idx = (np.arange(L, dtype=np.int64)[:, None] * np.arange(L, dtype=np.int64)[None, :]) % L
        base = 2.0 * np.pi * np.arange(L, dtype=np.float64) / L
        out['dftc' + name] = (np.cos(base)[idx] / np.sqrt(L)).astype(np.float32).astype(ml_dtypes.bfloat16)
        out['dfts' + name] = (np.sin(base)[idx] / np.sqrt(L)).astype(np.float32).astype(ml_dtypes.bfloat16)
        t = np.arange(L)
        ic = np.zeros((4, L), np.float32)
        for g, w in enumerate((2, 4, 8, 16)):
            lo = np.clip(t - w // 2, 0, L)
            hi = np.clip(t + w - w // 2, 0, L)
            ic[g] = 1.0 / (hi - lo)
        out['invcnt' + name] = ic
    ang = 2.0 * np.pi * ((np.arange(128)[:, None] * np.arange(128)[None, :]) % 128) / 128.0
    out['dft128'] = (np.concatenate([np.cos(ang), np.sin(ang)], axis=1) / np.sqrt(128.0)).astype(np.float32)
    out['ident'] = np.eye(128, dtype=np.float32)
    out['iota512'] = np.ascontiguousarray(np.broadcast_to(np.arange(512, dtype=np.float32), (128, 512)))
    out['jcol'] = (np.arange(4, dtype=np.float32)[None, :] * 128 + np.arange(128, dtype=np.float32)[:, None])
    _CONSTS = out
    return out


def make_biasx(rpb):
    a = np.arange(2)[:, None, None, None]
    kc = np.arange(64)[None, :, None, None]
    rl = np.arange(8)[None, None, :, None]
    c = np.arange(64)[None, None, None, :]
    cs = np.clip(c - 8, 0, 48)
    colok = (kc >= cs) & (kc < cs + 16)
    coff = np.clip(kc - c + 15, 0, 30)
    out = np.full((DEPTH, NH, 20, 2, 64, 8, 64), NEG, np.float32)
    for j in (0, 1, 7):
        tiles, base = chunk_tiles(j)
        for ti, m in enumerate(tiles):
            kr = 2 * m + a
            r = 8 * j + rl
            rs = np.clip(r - 4, 0, 56)
            rowok = (kr >= rs) & (kr <= rs + 7)
            roff = np.clip(kr - r + 7, 0, 14)
            ok = np.broadcast_to(rowok & colok, (2, 64, 8, 64))
            ro = np.broadcast_to(roff, (2, 64, 8, 64))
            co = np.broadcast_to(coff, (2, 64, 8, 64))
            vals = rpb[:, :, ro, co]
            out[:, :, base + ti] = np.where(ok[None, None], vals, np.float32(NEG))
    return np.ascontiguousarray(out.reshape(DEPTH * NH, 20, 128, 512))


def host_shared(inputs):
    sh = dict(make_consts())
    f = lambda k: np.ascontiguousarray(np.asarray(inputs[k], np.float32))
    sh['c_ctx'] = f('c_ctx').reshape(1, D)
    for k in ('ada_w', 'ada_b', 'norm1_g', 'norm2_g', 'w_in', 'w_att_o', 'w_fourier', 'w_pool', 'pool_scale',
              'w_out', 'w_router'):
        sh[k] = f(k)
    sh['w_g'] = f('w_exp_gate').reshape(DEPTH * NE, D, FF)
    sh['w_u'] = f('w_exp_up').reshape(DEPTH * NE, D, FF)
    sh['w_d'] = f('w_exp_down').reshape(DEPTH * NE, FF, D)
    sh['final_g'] = f('final_norm_g').reshape(1, D)
    sh['biasx'] = make_biasx(f('rpb'))
    return sh


def core_inputs(inputs, b, shared, names):
    m = {}
    for n in names:
        if n == 'xT':
            m[n] = np.ascontiguousarray(np.asarray(inputs['x'][b], np.float32).T)
        elif n == 'ctxT':
            m[n] = np.ascontiguousarray(np.asarray(inputs['ctx'][b], np.float32).T)
        elif n == 'c':
            m[n] = np.ascontiguousarray(np.asarray(inputs['c'][b], np.float32)).reshape(1, D)
        else:
            m[n] = shared[n]
    return m


def kernel(**inputs):
    nc, net = build_nc()
    shared = host_shared(inputs)
    names = list(net.ins.keys())
    in_maps = [core_inputs(inputs, b, shared, names) for b in range(8)]
    res = run_bass_kernel_spmd(nc, in_maps, core_ids=list(range(8)))
    out = np.stack([np.ascontiguousarray(r['outT'].T) for r in res.results], axis=0)
    return out.astype(np.float32)
```

```python
import numpy as np
from contextlib import ExitStack
import concourse.bass as bass
import concourse.mybir as mybir
from concourse.bass_utils import run_bass_kernel_spmd

F32 = mybir.dt.float32
F32R = mybir.dt.float32r
BF16 = mybir.dt.bfloat16
AF = mybir.ActivationFunctionType
ALU = mybir.AluOpType

D = 1024
T = 4096
TC = 256
DEPTH = 2
NH = 16
HD = 64
IN_W = 7168
K_OFF, V_OFF, Q_OFF, F_OFF, P_OFF, G_OFF = 0, 1024, 2048, 3072, 3584, 4096
NE = 16
FF = 2816
EPS = 1e-6
NEG = -30000.0
ENGS = ['pe', 'act', 'dve', 'pool', 'sp']


class Buf:
    __slots__ = ('name', 'last_w', 'readers', 'semslot', 'tracked')

    def __init__(self, name, tracked=True):
        self.name = name
        self.last_w = None
        self.readers = []
        self.semslot = None
        self.tracked = tracked


class V:
    __slots__ = ('buf', 'ap')

    def __init__(self, buf, ap):
        self.buf = buf
        self.ap = ap

    def __getitem__(self, idx):
        return V(self.buf, self.ap[idx])

    def r(self):
        return V(self.buf, self.ap.bitcast(F32R))

    def re(self, pat, **kw):
        return V(self.buf, self.ap.rearrange(pat, **kw))


class Op:
    __slots__ = ('eng', 'fn', 'kind', 'deps', 'signal', 'val', 'semslot', 'gidx', 'key')

    def __init__(self, eng, fn, kind):
        self.eng = eng
        self.fn = fn
        self.kind = kind
        self.deps = []
        self.signal = False
        self.val = None
        self.semslot = None
        self.gidx = 0
        self.key = 0


class Prog:
    def __init__(self, nc, stack, n_dma_sems=96):
        self.nc = nc
        self.eobj = {'pe': nc.tensor, 'act': nc.scalar, 'dve': nc.vector, 'pool': nc.gpsimd, 'sp': nc.sync}
        self.esem = {e: stack.enter_context(nc.semaphore('es_' + e)) for e in ENGS}
        self.ecount = {e: 0 for e in ENGS}
        self.slots = [[stack.enter_context(nc.semaphore('ds%d' % i)), 0] for i in range(n_dma_sems)]
        self.free_slots = list(range(n_dma_sems))
        self.ops = {e: [] for e in ENGS}
        self.waited = {e: {} for e in ENGS}
        self.phase_bufs = []
        self.pstack = None
        self.nphase = 0
        self.uid = 0
        self.gcount = 0

    def begin(self):
        self.pstack = ExitStack()
        self.pstack.__enter__()

    def _name(self, name):
        self.uid += 1
        return '%s_%d' % (name, self.uid)

    def tile(self, shape, dtype=F32, name='t', stack=None):
        st = stack or self.pstack
        h = st.enter_context(self.nc.sbuf_tensor(self._name(name), list(shape), dtype))
        b = Buf(name)
        self.phase_bufs.append(b)
        return V(b, h[:])

    def psum(self, shape=(128, 512), dtype=F32, name='ps'):
        h = self.pstack.enter_context(self.nc.psum_tensor(self._name(name), list(shape), dtype))
        b = Buf(name)
        self.phase_bufs.append(b)
        return V(b, h[:])

    def _record(self, op, reads, writes):
        deps = []
        for v in reads:
            b = v.buf
            if b.tracked and b.last_w is not None:
                deps.append(b.last_w)
        for v in writes:
            b = v.buf
            if b.tracked:
                if b.last_w is not None:
                    deps.append(b.last_w)
                deps.extend(b.readers)
        self.gcount += 1
        op.gidx = self.gcount
        for d in deps:
            if d is op:
                continue
            op.key = max(op.key, d.gidx if d.kind == 'c' else d.key)
            if d.kind == 'c':
                if d.eng == op.eng and d.eng == 'pe' and op.kind == 'c':
                    continue
                d.signal = True
            op.deps.append(d)
        for v in reads:
            if v.buf.tracked:
                v.buf.readers.append(op)
        for v in writes:
            if v.buf.tracked:
                v.buf.last_w = op
                v.buf.readers = []
        self.ops[op.eng].append(op)

    def op(self, eng, fn, reads=(), writes=()):
        o = Op(eng, fn, 'c')
        self._record(o, reads, writes)
        return o

    def dma(self, out, in_, q='sp', n=1, fn=None, **kw):
        sb = out.buf if out.buf.tracked else in_.buf
        assert sb.tracked
        if sb.semslot is None:
            sb.semslot = self.free_slots.pop()
        o = Op(q, None, 'd')
        o.semslot = sb.semslot
        slot = self.slots[sb.semslot]
        slot[1] += 16 * n
        o.val = slot[1]
        if fn is None:
            oap, iap = out.ap, in_.ap
            o.fn = lambda e: [e.dma_start(out=oap, in_=iap, **kw)]
        else:
            o.fn = fn
        self._record(o, [in_], [out])
        return o

    def dmar(self, out, in_, **kw):
        return self.dma(out.r(), in_.r(), **kw)

    def flush(self):
        nc = self.nc
        for e in ENGS:
            for o in self.ops[e]:
                if o.kind == 'c' and o.signal:
                    self.ecount[e] += 1
                    o.val = self.ecount[e]
        used_slots = set()
        for e in ENGS:
            for o in self.ops[e]:
                if o.kind == 'd':
                    used_slots.add(o.semslot)
        drain_eng = 'sp'
        with nc.Block() as block:
            secs = {'pe': block.tensor, 'act': block.scalar, 'dve': block.vector, 'pool': block.gpsimd,
                    'sp': block.sync}
            for e in ENGS:
                ops = self.ops[e]
                if not ops and e != drain_eng:
                    continue

                def body(eo, ops=ops, e=e):
                    waited = self.waited[e]
                    for o in ops:
                        for d in o.deps:
                            if d.kind == 'c':
                                key = ('e', d.eng)
                                sem = self.esem[d.eng]
                            else:
                                key = ('d', d.semslot)
                                sem = self.slots[d.semslot][0]
                            if waited.get(key, 0) >= d.val:
                                continue
                            eo.wait_ge(sem, d.val)
                            waited[key] = d.val
                        ins = o.fn(eo)
                        if o.kind == 'c':
                            if o.signal:
                                ins.then_inc(self.esem[e], 1)
                        else:
                            for i in ins:
                                i.then_inc(self.slots[o.semslot][0], 16)
                    if e == drain_eng:
                        for s in sorted(used_slots):
                            sem, cnt = self.slots[s]
                            if waited.get(('d', s), 0) < cnt:
                                eo.wait_ge(sem, cnt)
                                waited[('d', s)] = cnt

                secs[e](body)
        for b in self.phase_bufs:
            b.last_w = None
            b.readers = []
            if b.semslot is not None:
                self.free_slots.append(b.semslot)
                b.semslot = None
        self.phase_bufs = []
        self.ops = {e: [] for e in ENGS}
        self.pstack.__exit__(None, None, None)
        self.pstack = None
        self.nphase += 1

    def mm(self, out, lhsT, rhs, start, stop):
        o, l, r = out.ap, lhsT.ap, rhs.ap
        return self.op('pe', lambda e: e.matmul(o, l, r, start=start, stop=stop), [lhsT, rhs], [out])

    def transpose(self, out, in_, ident):
        o, i, d = out.ap, in_.ap, ident.ap
        return self.op('pe', lambda e: e.transpose(o, i, d), [in_, ident], [out])

    def act(self, out, in_, func, scale=1.0, bias=0.0, eng='act', extra_reads=()):
        o, i = out.ap, in_.ap
        sc = scale.ap if isinstance(scale, V) else scale
        bi = bias.ap if isinstance(bias, V) else bias
        rd = [in_] + [x for x in (scale, bias) if isinstance(x, V)] + list(extra_reads)
        return self.op('act', lambda e: e.activation(o, i, func, bias=bi, scale=sc), rd, [out])

    def ts(self, out, in0, s1, s2, op0, op1=None, eng='dve'):
        o, i = out.ap, in0.ap
        a1 = s1.ap if isinstance(s1, V) else s1
        a2 = s2.ap if isinstance(s2, V) else s2
        rd = [in0] + [x for x in (s1, s2) if isinstance(x, V)]
        if op1 is None:
            return self.op(eng, lambda e: e.tensor_scalar(o, i, a1, None, op0), rd, [out])
        return self.op(eng, lambda e: e.tensor_scalar(o, i, a1, a2, op0, op1), rd, [out])

    def tt(self, out, in0, in1, op, eng='dve'):
        o, a, b = out.ap, in0.ap, in1.ap
        return self.op(eng, lambda e: e.tensor_tensor(o, a, b, op), [in0, in1], [out])

    def stt(self, out, in0, scalar, in1, op0, op1):
        o, a, b = out.ap, in0.ap, in1.ap
        s = scalar.ap if isinstance(scalar, V) else scalar
        rd = [in0, in1] + ([scalar] if isinstance(scalar, V) else [])
        return self.op('dve', lambda e: e.scalar_tensor_tensor(o, a, s, b, op0, op1), rd, [out])

    def copy(self, out, in_, eng='dve'):
        o, i = out.ap, in_.ap
        if eng == 'act':
            return self.op('act', lambda e: e.copy(o, i), [in_], [out])
        return self.op(eng, lambda e: e.tensor_copy(o, i), [in_], [out])

    def recip(self, out, in_):
        o, i = out.ap, in_.ap
        return self.op('dve', lambda e: e.reciprocal(o, i), [in_], [out])

    def memset(self, out, val, eng='dve'):
        o = out.ap
        return self.op(eng, lambda e: e.memset(o, val), [], [out])


class Ring:
    def __init__(self, items):
        self.items = items
        self.i = 0

    def next(self):
        it = self.items[self.i % len(self.items)]
        self.i += 1
        return it


def dram(nc, name, shape, dtype=F32, kind='Internal'):
    h = nc.dram_tensor(name, list(shape), dtype, kind=kind)
    return V(Buf(name, tracked=False), h.ap())


class Net:
    def __init__(self, nc, stack, dbg=(), stop=None):
        self.nc = nc
        self.dbg = set(dbg)
        self.stop = stop
        self.p = Prog(nc, stack)
        self.stack = stack
        p = self.p
        self.in_shapes = {
            'xT': ([D, T], F32), 'c': ([1, D], F32), 'ctxT': ([D, TC], F32), 'c_ctx': ([1, D], F32),
            'ada_w': ([DEPTH, D, 6 * D], F32), 'ada_b': ([DEPTH, 6 * D], F32),
            'norm1_g': ([DEPTH, D], F32), 'norm2_g': ([DEPTH, D], F32), 'w_in': ([DEPTH, D, IN_W], F32),
            'biasx': ([DEPTH * NH, 20, 128, 512], F32), 'w_att_o': ([DEPTH, D, D], F32),
            'w_fourier': ([DEPTH, 512, D], F32), 'w_pool': ([DEPTH, 4, 128, 256], F32),
            'pool_scale': ([DEPTH, D], F32), 'w_out': ([DEPTH, D, D], F32), 'w_router': ([DEPTH, D, NE], F32),
            'w_g': ([DEPTH * NE, D, FF], F32), 'w_u': ([DEPTH * NE, D, FF], F32),
            'w_d': ([DEPTH * NE, FF, D], F32), 'final_g': ([1, D], F32),
            'dftc': ([T, T], BF16), 'dfts': ([T, T], BF16), 'dftc_c': ([TC, TC], BF16),
            'dfts_c': ([TC, TC], BF16), 'dft128': ([128, 256], F32), 'invcnt': ([4, T], F32),
            'invcnt_c': ([4, TC], F32), 'ident': ([128, 128], F32), 'iota512': ([128, 512], F32),
            'jcol': ([128, 4], F32)}
        self.ins = {}
        self.out = dram(nc, 'outT', [D, T], F32, 'ExternalOutput')
        self.scr = {}

    def __getattr__(self, name):
        d = self.__dict__
        if 'in_shapes' in d and name in d['in_shapes']:
            if name not in d['ins']:
                sh, dt = d['in_shapes'][name]
                d['ins'][name] = dram(d['nc'], name, sh, dt, 'ExternalInput')
            return d['ins'][name]
        raise AttributeError(name)

    def S(self, name, shape, dtype=F32):
        if name not in self.scr:
            kind = 'ExternalOutput' if name in self.dbg else 'Internal'
            self.scr[name] = dram(self.nc, name, shape, dtype, kind)
        return self.scr[name]

    def col_load(self, dst, src_row):
        p = self.p
        oap = dst.ap
        iap = src_row.ap.rearrange("o (k q) -> q (o k)", q=128)
        p.dma(dst, src_row, fn=lambda e: [e.dma_start(out=oap, in_=iap, allow_slow_non_contiguous=True)])

    def phase_adaln(self):
        p = self.p
        p.begin()
        mod = self.S('mod', [DEPTH * 2, 6 * D])
        craw = p.tile([128, 8, 2], F32, 'craw')
        s2 = p.tile([128, 8, 2], F32, 's2')
        self.col_load(craw[:, :, 0], self.c)
        self.col_load(craw[:, :, 1], self.c_ctx)
        p.act(s2, craw, AF.Silu)
        wb = Ring([p.tile([128, 8, 512], F32, 'adaw') for _ in range(6)])
        bb = Ring([p.tile([2, 512], F32, 'adab') for _ in range(6)])
        ob = Ring([p.tile([2, 512], F32, 'modo') for _ in range(2)])
        ps = Ring([p.psum([128, 512], F32, 'adaps') for _ in range(2)])
        lq = []

        def aload(i):
            l, nb = divmod(i, 12)
            w = wb.next()
            p.dma(w, self.ada_w[l, :, nb * 512:(nb + 1) * 512].re("(k q) n -> q k n", q=128))
            b = bb.next()
            p.dma(b, V(self.ada_b.buf, self.ada_b.ap[l:l + 1, nb * 512:(nb + 1) * 512].partition_broadcast(2)
                       .rearrange("p o n -> p (o n)")))
            lq.append((w, b))

        for i in range(4):
            aload(i)
        for l in range(DEPTH):
            for nb in range(12):
                i = l * 12 + nb
                if i + 4 < DEPTH * 12:
                    aload(i + 4)
                w, b = lq[i]
                acc = ps.next()
                for k in range(8):
                    p.mm(acc[0:2, :], s2[:, k, :], w[:, k, :], k == 0, k == 7)
                o = ob.next()
                p.tt(o, acc[0:2, :], b, ALU.add)
                p.dma(mod[2 * l:2 * l + 2, nb * 512:(nb + 1) * 512], o)
        p.flush()

    def load_modcols(self, l, si, seg, name):
        p = self.p
        mod = self.S('mod', [DEPTH * 2, 6 * D])
        t = p.tile([128, 8], F32, name)
        self.col_load(t, mod[2 * l + si:2 * l + si + 1, seg * D:(seg + 1) * D])
        return t

    def norm_consts(self, l, si, which):
        p = self.p
        base = 0 if which == 1 else 3
        sh = self.load_modcols(l, si, base + 0, 'sh')
        sc = self.load_modcols(l, si, base + 1, 'sc')
        g = p.tile([128, 8], F32, 'ng')
        ng = self.norm1_g if which == 1 else self.norm2_g
        self.col_load(g, ng[l:l + 1, :])
        gs = p.tile([128, 8], F32, 'gs')
        p.stt(gs, sc, 1.0, g, ALU.add, ALU.mult)
        return gs, sh

    def norm_load(self, xsrc, t0, nt, xring):
        xb = xring.next()
        self.p.dma(xb[:, :, 0:nt], xsrc[:, t0:t0 + nt].re("(k q) t -> q k t", q=128))
        return xb

    def emit_norm(self, xsrc, t0, nt, gs, sh, hT, hoff, ones, xring, sqring, tmpring, psring, rsring, xb=None):
        p = self.p
        if xb is None:
            xb = self.norm_load(xsrc, t0, nt, xring)
        acc = psring.next()
        for k in range(8):
            sq = sqring.next()
            p.act(sq[:, 0:nt], xb[:, k, 0:nt], AF.Square)
            p.mm(acc[:, 0:nt], ones, sq[:, 0:nt], k == 0, k == 7)
        rs = rsring.next()
        p.act(rs[:, 0:nt], acc[:, 0:nt], AF.Sqrt, scale=1.0 / D, bias=self.epsc)
        p.recip(rs[:, 0:nt], rs[:, 0:nt])
        for k in range(8):
            tmp = tmpring.next()
            p.tt(tmp[:, 0:nt], xb[:, k, 0:nt], rs[:, 0:nt], ALU.mult)
            p.act(hT[:, k, hoff:hoff + nt].r(), tmp[:, 0:nt], AF.Identity, scale=gs[:, k:k + 1], bias=sh[:, k:k + 1])

    def phase_normproj(self, l, xsrc, cxsrc, full_ctx):
        p = self.p
        dst = {}
        for si, Tn in ((0, T), (1, TC)):
            sfx = '%d_%d' % (l, si)
            d = {'k': self.S('kT' + sfx, [D, Tn], BF16), 'v': self.S('v' + sfx, [Tn, D], BF16)}
            if si == 0 or full_ctx:
                d['q'] = self.S('qT' + sfx, [D, Tn], BF16)
                d['u'] = self.S('uT' + sfx, [512, Tn])
                d['p'] = self.S('pT' + sfx, [512, Tn])
                d['g'] = self.S('gT' + sfx, [3 * D, Tn])
            dst[si] = d
        TBIG = 2048
        p.begin()
        cons = {0: self.norm_consts(l, 0, 1), 1: self.norm_consts(l, 1, 1)}
        srcs = {0: xsrc, 1: cxsrc}
        ones = p.tile([128, 128], F32, 'ones')
        p.memset(ones, 1.0)
        self.epsc = p.tile([128, 1], F32, 'epsc')
        p.memset(self.epsc, EPS)
        hT = p.tile([128, 8, TBIG + TC], F32, 'hT')
        xring = Ring([p.tile([128, 8, 512], F32, 'xb') for _ in range(2)])
        sqring = Ring([p.tile([128, 512], F32, 'sq') for _ in range(3)])
        tmpring = Ring([p.tile([128, 512], F32, 'tmp') for _ in range(3)])
        rsring = Ring([p.tile([128, 512], F32, 'rs') for _ in range(2)])
        psn = Ring([p.psum([128, 512], F32, 'psn') for _ in range(2)])
        psm = Ring([p.psum([128, 512], F32, 'psm') for _ in range(6)])
        wring = Ring([p.tile([128, 8, 512], F32, 'wblk') for _ in range(3)])
        o32 = Ring([p.tile([128, 512], F32, 'o32') for _ in range(8)])
        o16 = Ring([p.tile([128, 512], BF16, 'o16') for _ in range(10)])
        ev = 0
        passes = [[(0, 0, TBIG, 0), (1, 0, TC, TBIG)], [(0, TBIG, TBIG, 0)]]
        for segs in passes:
            for si, t0, n, ho in segs:
                nb = min(512, n)
                for bi in range(n // nb):
                    self.emit_norm(srcs[si], t0 + bi * nb, nb, cons[si][0], cons[si][1], hT, ho + bi * nb, ones,
                                   xring, sqring, tmpring, psn, rsring)
            wq = []

            def wload(cb):
                w = wring.next()
                p.dmar(w, self.w_in[l, :, cb * 512:(cb + 1) * 512].re("(k q) n -> q k n", q=128))
                wq.append(w)

            wload(0)
            wload(1)
            for cb in range(14):
                if cb + 2 < 14:
                    wload(cb + 2)
                w = wq[cb]
                c0 = cb * 512
                for si, t0, n, ho in segs:
                    d = dst[si]
                    if si == 1 and not full_ctx and c0 >= Q_OFF:
                        continue
                    nb = min(512, n)
                    if V_OFF <= c0 < Q_OFF:
                        for tt_ in range(n // 128):
                            acc = psm.next()
                            for k in range(8):
                                p.mm(acc, hT[:, k, ho + tt_ * 128:ho + (tt_ + 1) * 128].r(), w[:, k, :].r(),
                                     k == 0, k == 7)
                            o = o16.next()
                            ev += 1
                            p.copy(o, acc, eng='act' if ev % 2 else 'dve')
                            tok0 = t0 + tt_ * 128
                            p.dma(d['v'][tok0:tok0 + 128, c0 - V_OFF:c0 - V_OFF + 512], o)
                        continue
                    for ch in range(4):
                        col = c0 + ch * 128
                        for bi in range(n // nb):
                            acc = psm.next()
                            for k in range(8):
                                p.mm(acc[:, 0:nb], w[:, k, ch * 128:(ch + 1) * 128].r(),
                                     hT[:, k, ho + bi * nb:ho + (bi + 1) * nb].r(), k == 0, k == 7)
                            tok0 = t0 + bi * nb
                            ev += 1
                            if col < V_OFF:
                                o = o16.next()
                                p.copy(o[:, 0:nb], acc[:, 0:nb], eng='act' if ev % 2 else 'dve')
                                p.dma(d['k'][col:col + 128, tok0:tok0 + nb], o[:, 0:nb])
                            elif col < F_OFF:
                                o = o16.next()
                                if ev % 2:
                                    p.act(o[:, 0:nb], acc[:, 0:nb], AF.Copy, scale=HD ** -0.5)
                                else:
                                    p.ts(o[:, 0:nb], acc[:, 0:nb], HD ** -0.5, None, ALU.mult)
                                p.dma(d['q'][col - Q_OFF:col - Q_OFF + 128, tok0:tok0 + nb], o[:, 0:nb])
                            elif col < G_OFF:
                                o = o32.next()
                                p.copy(o[:, 0:nb], acc[:, 0:nb], eng='act' if ev % 2 else 'dve')
                                dd = d['u'] if col < P_OFF else d['p']
                                r0 = (col - F_OFF) % 512
                                p.dma(dd[r0:r0 + 128, tok0:tok0 + nb], o[:, 0:nb])
                            else:
                                o = o32.next()
                                p.act(o[:, 0:nb], acc[:, 0:nb], AF.Sigmoid)
                                p.dma(d['g'][col - G_OFF:col - G_OFF + 128, tok0:tok0 + nb], o[:, 0:nb])
        p.flush()

    def phase_attn(self, l, with_ctxq):
        p = self.p
        kT = self.S('kT%d_0' % l, [D, T], BF16)
        qT = self.S('qT%d_0' % l, [D, T], BF16)
        vv = self.S('v%d_0' % l, [T, D], BF16)
        kTc = self.S('kT%d_1' % l, [D, TC], BF16)
        vc = self.S('v%d_1' % l, [TC, D], BF16)
        attT = self.S('attT%d_0' % l, [D, T])
        if with_ctxq:
            qTc = self.S('qT%d_1' % l, [D, TC], BF16)
            attTc = self.S('attT%d_1' % l, [D, TC])
        p.begin()
        vall = p.tile([128, 32, D], BF16, 'vall')
        p.dma(vall, vv.re("(i q) d -> q i d", q=128))
        vcall = p.tile([128, 2, D], BF16, 'vcall')
        p.dma(vcall, vc.re("(i q) d -> q i d", q=128))
        idf = p.tile([128, 128], F32, 'idf')
        p.dma(idf, self.ident)
        idb = p.tile([128, 128], BF16, 'idb')
        p.copy(idb, idf)
        onesb = p.tile([128, 128], BF16, 'onesb2')
        p.memset(onesb, 1.0)
        khz = [p.tile([128, T], BF16, 'khz') for _ in range(2)]
        kcz = [p.tile([128, TC], BF16, 'kcz') for _ in range(2)]
        for par in range(2):
            z = slice(64, 128) if par == 0 else slice(0, 64)
            p.memset(khz[par][z, :], 0.0, eng='pool')
            p.memset(kcz[par][z, :], 0.0, eng='pool')
        qpr = Ring([p.tile([128, T], BF16, 'qp') for _ in range(2)])
        qcr = Ring([p.tile([128, TC], BF16, 'qcp') for _ in range(2)])
        bhr = Ring([p.tile([128, 20, 512], BF16, 'bh') for _ in range(2)])
        psS = Ring([p.psum([128, 512], F32, 'psS') for _ in range(4)])
        psN = Ring([p.psum([128, 512], F32, 'psN') for _ in range(2)])
        psD = Ring([p.psum([128, 512], F32, 'psD') for _ in range(2)])
        pr = Ring([p.tile([128, 512], BF16, 'pt') for _ in range(6)])
        rcr = Ring([p.tile([128, 512], F32, 'rc') for _ in range(2)])
        aor = Ring([p.tile([128, 512], F32, 'ao') for _ in range(3)])
        hq = {}

        def hload(h):
            hs = slice(h * 64, (h + 1) * 64)
            par = h % 2
            hp = slice(par * 64, par * 64 + 64)
            ps_ = slice((h // 2) * 128, (h // 2 + 1) * 128)
            p.dma(khz[par][hp, :], kT[hs, :])
            p.dma(kcz[par][hp, :], kTc[hs, :])
            if par == 0:
                qh = qpr.next()
                p.dma(qh, qT[ps_, :])
                qch = None
                if with_ctxq:
                    qch = qcr.next()
                    p.dma(qch, qTc[ps_, :])
                hq[h // 2] = (qh, qch)
            bh = bhr.next()
            p.dma(bh, self.biasx[l * NH + h].re("n q f -> q n f"), q='pool')
            hq[('b', h)] = bh

        hload(0)
        for h in range(NH):
            hs = slice(h * 64, (h + 1) * 64)
            par = h % 2
            hp = slice(par * 64, par * 64 + 64)
            ps_ = slice((h // 2) * 128, (h // 2 + 1) * 128)
            if h + 1 < NH:
                hload(h + 1)
            kh = khz[par]
            kch = kcz[par]
            qh, qch = hq[h // 2]
            bh = hq[('b', h)]
            jobs = [(j, 512) for j in range(8)] + ([(-1, TC)] if with_ctxq else [])
            for j, nq in jobs:
                if j >= 0:
                    tiles, base = chunk_tiles(j)
                    qv = qh[:, j * 512:(j + 1) * 512]
                else:
                    tiles, base = [], 0
                    qv = qch[:, 0:TC]
                num = psN.next()
                den = psD.next()
                ntot = len(tiles) + 2
                pend = []

                def pv(item, num=num, den=den, nq=nq, ntot=ntot):
                    ti, vt, pt = item
                    p.mm(num[:, 0:nq], vt, pt[:, 0:nq], ti == 0, ti == ntot - 1)
                    p.mm(den[:, 0:nq], onesb, pt[:, 0:nq], ti == 0, ti == ntot - 1)

                for ti in range(ntot):
                    S_ = psS.next()
                    if ti < len(tiles):
                        m = tiles[ti]
                        p.mm(S_[:, 0:nq], kh[:, m * 128:(m + 1) * 128], qv, True, False)
                        p.mm(S_[:, 0:nq], idb, bh[:, base + ti, :], False, True)
                        vt = vall[:, m, ps_]
                    else:
                        cm = ti - len(tiles)
                        p.mm(S_[:, 0:nq], kch[:, cm * 128:(cm + 1) * 128], qv, True, True)
                        vt = vcall[:, cm, ps_]
                    pt = pr.next()
                    p.act(pt[:, 0:nq], S_[:, 0:nq], AF.Exp)
                    pend.append((ti, vt, pt))
                    if len(pend) > 2:
                        pv(pend.pop(0))
                while pend:
                    pv(pend.pop(0))
                rc = rcr.next()
                p.recip(rc[hp, 0:nq], den[hp, 0:nq])
                ao = aor.next()
                p.tt(ao[hp, 0:nq], num[hp, 0:nq], rc[hp, 0:nq], ALU.mult)
                if j >= 0:
                    p.dma(attT[hs, j * 512:(j + 1) * 512], ao[hp, :])
                else:
                    p.dma(attTc[hs, 0:TC], ao[hp, 0:TC])
        p.flush()

    def phase_fourier_pool(self, l, si):
        p = self.p
        Tn = T if si == 0 else TC
        NI = Tn // 128
        NBk = min(1024, Tn)
        nh = (NBk + 511) // 512
        hw = min(512, NBk)
        sfx = '%d_%d' % (l, si)
        uT = self.S('uT' + sfx, [512, Tn])
        pT = self.S('pT' + sfx, [512, Tn])
        fourT = self.S('fourT' + sfx, [512, Tn])
        poolT = self.S('poolT' + sfx, [512, Tn])
        tabs = (self.dftc, self.dfts) if si == 0 else (self.dftc_c, self.dfts_c)
        icn = self.invcnt if si == 0 else self.invcnt_c
        p.begin()
        d128 = p.tile([128, 256], F32, 'd128')
        p.dmar(d128, self.dft128)
        AB = p.tile([128, NI, 4, 256], BF16, 'AB')
        ub = p.tile([128, Tn], F32, 'ub')
        acc = [[p.psum([128, 512], F32, 'facc') for _ in range(nh)] for _ in range(4)]
        ps1 = Ring([acc[0][0], acc[1][0]])
        pring = Ring([p.tile([128, 8, NBk], BF16, 'dpc') for _ in range(3)])
        o32 = Ring([p.tile([128, 512], F32, 'fo') for _ in range(4)])
        L = Tn + 32
        U = p.tile([128, L], F32, 'pU')
        A = p.tile([128, L], F32, 'pA')
        B = p.tile([128, L], F32, 'pB')
        iclo = p.tile([128, 16], F32, 'iclo')
        ichi = p.tile([128, 16], F32, 'ichi')
        for g in range(4):
            w = 2 << g
            half = w // 2
            eng = 'pool'
            p.memset(U[:, 0:16], 0.0, eng=eng)
            p.memset(U[:, 16 + Tn:L], 0.0, eng=eng)
            p.dma(U[:, 16:16 + Tn], pT[g * 128:(g + 1) * 128, :], q='pool')
            p.dma(iclo, V(icn.buf, icn.ap[g:g + 1, 0:16].partition_broadcast(128).rearrange("p o n -> p (o n)")), q='pool')
            p.dma(ichi, V(icn.buf, icn.ap[g:g + 1, Tn - 16:Tn].partition_broadcast(128)
                          .rearrange("p o n -> p (o n)")), q='pool')
            cur, n, bufs, bi = U, 1, [A, B], 0
            while n < w:
                nxt = bufs[bi]
                bi ^= 1
                ln = L - (2 * n - 1)
                p.tt(nxt[:, 0:ln], cur[:, 0:ln], cur[:, n:n + ln], ALU.add, eng=eng)
                cur = nxt
                n *= 2
            res = bufs[bi]
            s0 = 16 - half
            p.ts(res[:, 0:Tn], cur[:, s0:s0 + Tn], 1.0 / w, None, ALU.mult, eng=eng)
            p.tt(res[:, 0:16], cur[:, s0:s0 + 16], iclo, ALU.mult, eng=eng)
            p.tt(res[:, Tn - 16:Tn], cur[:, s0 + Tn - 16:s0 + Tn], ichi, ALU.mult, eng=eng)
            p.tt(res[:, 0:Tn], res[:, 0:Tn], U[:, 16:16 + Tn], ALU.subtract, eng=eng)
            p.dma(poolT[g * 128:(g + 1) * 128, :], res[:, 0:Tn], q='pool')
        ev = 0
        for g in range(4):
            p.dmar(ub, uT[g * 128:(g + 1) * 128, :])
            for i in range(NI):
                ps = ps1.next()
                p.mm(ps[:, 0:256], ub[:, i * 128:(i + 1) * 128].r(), d128.r(), True, True)
                p.copy(AB[:, i, g, 0:128], ps[:, 0:128], eng='act')
                p.ts(AB[:, i, g, 128:256], ps[:, 128:256], -1.0, None, ALU.mult)
        for kb in range(Tn // NBk):
            for ti, tbl in enumerate(tabs):
                for lg in range(max(1, NI // 8)):
                    nl = min(8, NI)
                    pc = pring.next()
                    p.dma(pc[:, 0:nl, :], tbl[lg * 1024:lg * 1024 + nl * 128, kb * NBk:(kb + 1) * NBk]
                          .re("(i q) k -> q i k", q=128))
                    for ii in range(nl):
                        i = lg * 8 + ii
                        for g in range(4):
                            for h in range(nh):
                                p.mm(acc[g][h][:, 0:hw], AB[:, i, g, ti * 128:(ti + 1) * 128],
                                     pc[:, ii, h * 512:h * 512 + hw], ti == 0 and i == 0, ti == 1 and i == NI - 1)
            for g in range(4):
                for h in range(nh):
                    o = o32.next()
                    ev += 1
                    p.copy(o[:, 0:hw], acc[g][h][:, 0:hw], eng='act' if ev % 2 else 'dve')
                    c0 = kb * NBk + h * 512
                    p.dma(fourT[g * 128:(g + 1) * 128, c0:c0 + hw], o[:, 0:hw])
        p.flush()

    def phase_merge(self, l, si, xsrc, xdst):
        p = self.p
        Tn = T if si == 0 else TC
        NB = min(512, Tn)
        sfx = '%d_%d' % (l, si)
        attT = self.S('attT' + sfx, [D, Tn])
        fourT = self.S('fourT' + sfx, [512, Tn])
        poolT = self.S('poolT' + sfx, [512, Tn])
        gT = self.S('gT' + sfx, [3 * D, Tn])
        p.begin()
        wa = p.tile([128, 8, D], F32, 'wa')
        p.dmar(wa, self.w_att_o[l].re("(k q) n -> q k n", q=128))
        wf = p.tile([128, 4, D], F32, 'wf')
        p.dmar(wf, self.w_fourier[l].re("(k q) n -> q k n", q=128))
        wp = p.tile([128, 4, 256], F32, 'wp')
        p.dmar(wp, self.w_pool[l].re("g c f -> c g f"))
        wo = p.tile([128, 8, D], F32, 'wo')
        p.dmar(wo, self.w_out[l].re("(k q) n -> q k n", q=128))
        g1 = self.load_modcols(l, si, 2, 'g1')
        psc = p.tile([128, 8], F32, 'psc')
        self.col_load(psc, self.pool_scale[l:l + 1, :])
        at = p.tile([128, 8, NB], F32, 'at')
        fo = p.tile([128, 4, NB], F32, 'fo')
        po = p.tile([128, 4, NB], F32, 'po')
        xbr = Ring([p.tile([128, 8, NB], F32, 'xb') for _ in range(2)])
        mg = p.tile([128, 8, NB], F32, 'mg')
        gring = Ring([p.tile([128, 3, NB], F32, 'gt') for _ in range(2)])
        tmp = Ring([p.tile([128, NB], F32, 'mt') for _ in range(6)])
        o32 = Ring([p.tile([128, NB], F32, 'xo') for _ in range(4)])
        psa = Ring([p.psum([128, 512], F32, 'psa') for _ in range(2)])
        psf = Ring([p.psum([128, 512], F32, 'psf') for _ in range(2)])
        psp = Ring([p.psum([128, 512], F32, 'psp') for _ in range(2)])
        pso = Ring([p.psum([128, 512], F32, 'pso') for _ in range(2)])
        gview = gT.re("(b k q) t -> q b k t", b=3, q=128)
        def load_in(tb):
            t0 = tb * NB
            p.dmar(at, attT[:, t0:t0 + NB].re("(k q) t -> q k t", q=128))
            p.dmar(fo, fourT[:, t0:t0 + NB].re("(k q) t -> q k t", q=128))
            p.dmar(po, poolT[:, t0:t0 + NB].re("(k q) t -> q k t", q=128))

        ntb = Tn // NB
        gq = {}

        def gload(i):
            if i >= ntb * 8 or i in gq:
                return
            tb_, dc_ = divmod(i, 8)
            g = gring.next()
            p.dma(g, gview[:, :, dc_, tb_ * NB:(tb_ + 1) * NB])
            gq[i] = g

        load_in(0)
        gload(0)
        for tb in range(Tn // NB):
            t0 = tb * NB
            xb = xbr.next()
            p.dma(xb, xsrc[:, t0:t0 + NB].re("(k q) t -> q k t", q=128))
            for dc in range(8):
                cs = slice(dc * 128, (dc + 1) * 128)
                gload(tb * 8 + dc + 1)
                gt = gq[tb * 8 + dc]
                ya = psa.next()
                for k in range(8):
                    p.mm(ya[:, 0:NB], wa[:, k, cs].r(), at[:, k, :].r(), k == 0, k == 7)
                yf = psf.next()
                for k in range(4):
                    p.mm(yf[:, 0:NB], wf[:, k, cs].r(), fo[:, k, :].r(), k == 0, k == 3)
                yp = psp.next()
                p.mm(yp[:, 0:NB], wp[:, dc // 2, (dc % 2) * 128:(dc % 2 + 1) * 128].r(), po[:, dc // 2, :].r(),
                     True, True)
                m1, m2, m3 = tmp.next(), tmp.next(), tmp.next()
                p.tt(m1, ya[:, 0:NB], gt[:, 0, :], ALU.mult)
                p.tt(m2, yf[:, 0:NB], gt[:, 1, :], ALU.mult)
                p.stt(m3, yp[:, 0:NB], psc[:, dc:dc + 1], gt[:, 2, :], ALU.mult, ALU.mult)
                p.tt(m1, m1, m2, ALU.add, eng='pool')
                p.tt(mg[:, dc, :].r(), m1, m3, ALU.add)
            if tb + 1 < Tn // NB:
                load_in(tb + 1)
            for dc in range(8):
                cs = slice(dc * 128, (dc + 1) * 128)
                yo = pso.next()
                for k in range(8):
                    p.mm(yo[:, 0:NB], wo[:, k, cs].r(), mg[:, k, :].r(), k == 0, k == 7)
                xo = o32.next()
                p.stt(xo, yo[:, 0:NB], g1[:, dc:dc + 1], xb[:, dc, :], ALU.mult, ALU.add)
                p.dma(xdst[cs, t0:t0 + NB], xo)
        p.flush()

    def phase_route(self, l, si, xsrc):
        p = self.p
        Tn = T if si == 0 else TC
        NI = Tn // 128
        NB = min(512, Tn)
        cap = 2 * Tn // NE
        sfx = '%d_%d' % (l, si)
        h2 = self.S('h2tok' + sfx, [Tn, D], BF16)
        posT = self.S('posT' + sfx, [NE, Tn])
        affTd = self.S('affT' + sfx, [NE, Tn])
        postm = self.S('postm' + sfx, [128, NI * NE])
        p.begin()
        gs, sh = self.norm_consts(l, si, 2)
        ones = p.tile([128, 128], F32, 'ones')
        p.memset(ones, 1.0)
        self.epsc = p.tile([128, 1], F32, 'epsc')
        p.memset(self.epsc, EPS)
        idf = p.tile([128, 128], F32, 'idf')
        p.dma(idf, self.ident)
        wr = p.tile([128, 8, NE], F32, 'wr')
        p.dma(wr, self.w_router[l].re("(k q) e -> q k e", q=128))
        eT = p.tile([NE, Tn], F32, 'eT')
        affT = p.tile([NE, Tn], F32, 'affT')
        hring = Ring([p.tile([128, 8, NB], F32, 'h2T') for _ in range(2)])
        xring = Ring([p.tile([128, 8, NB], F32, 'xb') for _ in range(2)])
        sqring = Ring([p.tile([128, NB], F32, 'sq') for _ in range(3)])
        tmpring = Ring([p.tile([128, NB], F32, 'tmp') for _ in range(3)])
        rsring = Ring([p.tile([128, NB], F32, 'rs') for _ in range(2)])
        htr = Ring([p.tile([128, D], BF16, 'htok') for _ in range(3)])
        psn = Ring([p.psum([128, 512], F32, 'psn') for _ in range(2)])
        psl = Ring([p.psum([128, 512], F32, 'psl') for _ in range(2)])
        pst = Ring([p.psum([128, 512], F32, 'pst') for _ in range(4)])
        ev = 0
        xq = {0: self.norm_load(xsrc, 0, NB, xring)}
        for bi in range(Tn // NB):
            hT = hring.next()
            if bi + 1 < Tn // NB:
                xq[bi + 1] = self.norm_load(xsrc, (bi + 1) * NB, NB, xring)
            self.emit_norm(xsrc, bi * NB, NB, gs, sh, hT, 0, ones, xring, sqring, tmpring, psn, rsring, xb=xq[bi])
            lg = psl.next()
            for k in range(8):
                p.mm(lg[0:NE, 0:NB], wr[:, k, :], hT[:, k, :], k == 0, k == 7)
            p.act(eT[:, bi * NB:(bi + 1) * NB], lg[0:NE, 0:NB], AF.Exp)
            for tt_ in range(NB // 128):
                ht = htr.next()
                for hf in range(2):
                    pt = pst.next()
                    for kk in range(4):
                        k = hf * 4 + kk
                        p.transpose(pt[:, kk * 128:(kk + 1) * 128], hT[:, k, tt_ * 128:(tt_ + 1) * 128], idf)
                    ev += 1
                    p.copy(ht[:, hf * 512:(hf + 1) * 512], pt, eng='act' if ev % 2 else 'dve')
                tok0 = bi * NB + tt_ * 128
                p.dma(h2[tok0:tok0 + 128, :], ht)
        rcs = Ring([p.tile([NE, NB], F32, 'rcs') for _ in range(2)])
        for bi in range(Tn // NB):
            bs = slice(bi * NB, (bi + 1) * NB)
            sm = psl.next()
            p.mm(sm[0:NE, 0:NB], ones[0:NE, 0:NE], eT[:, bs], True, True)
            rc = rcs.next()
            p.recip(rc, sm[0:NE, 0:NB])
            p.tt(affT[:, bs], eT[:, bs], rc, ALU.mult)
        lo = p.tile([NE, 1], F32, 'lo')
        hi = p.tile([NE, 1], F32, 'hi')
        mid = p.tile([NE, 1], F32, 'mid')
        cnt = p.tile([NE, 1], F32, 'cnt')
        ge = p.tile([NE, 1], F32, 'ge')
        d1 = p.tile([NE, 1], F32, 'd1')
        junk = p.tile([NE, Tn], F32, 'junk')
        p.memset(lo, 0.0)
        p.memset(hi, 1.0)
        for it in range(30):
            p.ts(mid, lo, hi, 0.5, ALU.add, ALU.mult)
            ja, aa, ma, ca = junk.ap, affT.ap, mid.ap, cnt.ap
            p.op('dve', lambda e, ja=ja, aa=aa, ma=ma, ca=ca: e.tensor_scalar(ja, aa, ma, None, ALU.is_ge, ALU.add,
                                                                               accum_out=ca),
                 [affT, mid], [junk, cnt])
            p.ts(ge, cnt, float(cap), None, ALU.is_ge)
            p.tt(d1, mid, lo, ALU.subtract)
            p.stt(lo, d1, ge, lo, ALU.mult, ALU.add)
            p.tt(d1, hi, mid, ALU.subtract)
            p.stt(hi, d1, ge, mid, ALU.mult, ALU.add)
        mask = p.tile([NE, Tn], F32, 'mask')
        p.ts(mask, affT, lo, None, ALU.is_ge)
        p.memset(junk, 1.0)
        cum = p.tile([NE, Tn], F32, 'cum')
        ca, ja, ma = cum.ap, junk.ap, mask.ap
        p.op('dve', lambda e: e.tensor_tensor_scan(ca, ja, ma, 0.0, ALU.mult, ALU.add), [junk, mask], [cum])
        p.tt(cum, cum, mask, ALU.mult)
        p.ts(cum, cum, -1.0, None, ALU.add)
        p.dma(posT, cum)
        p.dma(affTd, affT)
        ptm = p.tile([128, NI * NE], F32, 'ptm')
        pt = pst.next()
        for i in range(NI):
            p.transpose(pt[:, i * NE:(i + 1) * NE], cum[:, i * 128:(i + 1) * 128], idf[0:NE, 0:NE])
        p.copy(ptm, pt[:, 0:NI * NE])
        p.dma(postm, ptm)
        p.flush()

    def phase_gather(self, l, streams):
        p = self.p
        p.begin()
        iota = p.tile([128, 512], F32, 'iota')
        p.dma(iota, self.iota512)
        acc = [p.psum([128, 512], F32, 'gacc') for _ in range(8)]
        sring = Ring([p.tile([128, 512], BF16, 'S') for _ in range(4)])
        xor_ = Ring([p.tile([128, 8, 512], F32, 'xgo') for _ in range(2)])
        ev = 0
        for si in streams:
            Tn = T if si == 0 else TC
            NI = Tn // 128
            cap = 2 * Tn // NE
            sfx = '%d_%d' % (l, si)
            h2 = self.S('h2tok' + sfx, [Tn, D], BF16)
            postm = self.S('postm' + sfx, [128, NI * NE])
            xg = self.S('xg' + sfx, [NE, D, cap])
            h2tok = p.tile([128, NI, D], BF16, 'h2tok')
            p.dma(h2tok, h2.re("(i q) d -> q i d", q=128))
            ptm = p.tile([128, NI, NE], F32, 'ptm')
            p.dma(ptm, postm.re("q (i e) -> q i e", e=NE))
            for e in range(NE):
                for i in range(NI):
                    S_ = sring.next()
                    p.ts(S_[:, 0:cap], iota[:, 0:cap], ptm[:, i, e:e + 1], None, ALU.is_equal)
                    for k in range(8):
                        p.mm(acc[k][:, 0:cap], h2tok[:, i, k * 128:(k + 1) * 128], S_[:, 0:cap], i == 0, i == NI - 1)
                xo = xor_.next()
                for k in range(8):
                    ev += 1
                    p.copy(xo[:, k, 0:cap], acc[k][:, 0:cap], eng='act' if ev % 2 else 'dve')
                p.dma(xg[e].re("(k q) c -> q k c", q=128), xo[:, :, 0:cap])
        p.flush()

    def phase_experts(self, l, streams):
        p = self.p
        p.begin()
        info = {}
        for si in streams:
            Tn = T if si == 0 else TC
            cap = 2 * Tn // NE
            Mp = cap if cap >= 128 else 128
            sfx = '%d_%d' % (l, si)
            xg = self.S('xg' + sfx, [NE, D, cap])
            ysc = self.S('y' + sfx, [NE, Mp, D], BF16)
            hT = p.tile([128, FF // 128, Mp], F32, 'ehT')
            if Mp != cap:
                p.memset(hT, 0.0)
            xr = Ring([p.tile([128, 8, cap], F32, 'exg') for _ in range(2)])
            yb = [p.psum([128, 512], F32, 'ey') for _ in range(Mp // 128)]
            info[si] = (cap, Mp, xg, ysc, hT, xr, yb)
        wgr = Ring([p.tile([128, 8, 512], F32, 'wg') for _ in range(2)])
        wur = Ring([p.tile([128, 8, 512], F32, 'wu') for _ in range(2)])
        wdr = Ring([p.tile([128, 4, 512], F32, 'wd') for _ in range(3)])
        npa = 8 - sum(len(info[si][6]) for si in streams)
        psa = Ring([p.psum([128, 512], F32, 'ea') for _ in range(max(1, npa - npa // 2))])
        psu = Ring([p.psum([128, 512], F32, 'eu') for _ in range(max(1, npa // 2))])
        slr = Ring([p.tile([128, 512], F32, 'sl') for _ in range(2)])
        yor = Ring([p.tile([128, 512], BF16, 'yo') for _ in range(5)])
        NFB = (FF + 511) // 512
        ev = 0
        xsq, guq, dq = {}, {}, {}

        def xs_load(e):
            if e >= NE or e in xsq:
                return
            xs = {}
            for si in streams:
                cap, Mp, xg, ysc, hT, xr, yb = info[si]
                xs[si] = xr.next()
                p.dmar(xs[si], xg[e].re("(k q) c -> q k c", q=128))
            xsq[e] = xs

        def gu_load(e, fb):
            if e >= NE or fb >= NFB or (e, fb) in guq:
                return
            ge = l * NE + e
            f0 = fb * 512
            fw = min(512, FF - f0)
            wgb = wgr.next()
            p.dmar(wgb[:, :, 0:fw], self.w_g[ge, :, f0:f0 + fw].re("(k q) f -> q k f", q=128))
            wub = wur.next()
            p.dmar(wub[:, :, 0:fw], self.w_u[ge, :, f0:f0 + fw].re("(k q) f -> q k f", q=128))
            guq[(e, fb)] = (wgb, wub)

        def d_load(e, dh, fb):
            if e >= NE or fb >= NFB or (e, dh, fb) in dq:
                return
            ge = l * NE + e
            f0 = fb * 512
            fw = min(512, FF - f0)
            wdb = wdr.next()
            p.dmar(wdb[:, 0:fw // 128, :], self.w_d[ge, f0:f0 + fw, dh * 512:(dh + 1) * 512]
                   .re("(c q) d -> q c d", q=128))
            dq[(e, dh, fb)] = wdb

        xs_load(0)
        gu_load(0, 0)
        for e in range(NE):
            xs = xsq[e]
            for fb in range(NFB):
                fw = min(512, FF - fb * 512)
                gu_load(e, fb)
                gu_load(e, fb + 1)
                wgb, wub = guq[(e, fb)]
                for fcl in range(fw // 128):
                    fc = fb * 4 + fcl
                    fs = slice(fcl * 128, (fcl + 1) * 128)
                    for si in streams:
                        cap, Mp, xg, ysc, hT, xr, yb = info[si]
                        a = psa.next()
                        for k in range(8):
                            p.mm(a[:, 0:cap], wgb[:, k, fs].r(), xs[si][:, k, :].r(), k == 0, k == 7)
                        u = psu.next()
                        for k in range(8):
                            p.mm(u[:, 0:cap], wub[:, k, fs].r(), xs[si][:, k, :].r(), k == 0, k == 7)
                        sl = slr.next()
                        p.act(sl[:, 0:cap], a[:, 0:cap], AF.Silu)
                        p.tt(hT[:, fc, 0:cap].r(), u[:, 0:cap], sl[:, 0:cap], ALU.mult)
                if fb == NFB - 2:
                    d_load(e, 0, 0)
            for dh in range(2):
                for fb in range(NFB):
                    fw = min(512, FF - fb * 512)
                    d_load(e, dh, fb)
                    d_load(e, dh, fb + 1)
                    wdb = dq[(e, dh, fb)]
                    for fcl in range(fw // 128):
                        fc = fb * 4 + fcl
                        for si in streams:
                            cap, Mp, xg, ysc, hT, xr, yb = info[si]
                            for sc in range(Mp // 128):
                                p.mm(yb[sc], hT[:, fc, sc * 128:(sc + 1) * 128].r(), wdb[:, fcl, :].r(),
                                     fc == 0, fc == FF // 128 - 1)
                if dh == 0:
                    d_load(e, 1, 0)
                    d_load(e, 1, 1)
                else:
                    xs_load(e + 1)
                    gu_load(e + 1, 0)
                    gu_load(e + 1, 1)
                for si in streams:
                    cap, Mp, xg, ysc, hT, xr, yb = info[si]
                    for sc in range(Mp // 128):
                        o = yor.next()
                        ev += 1
                        p.copy(o, yb[sc], eng='act' if ev % 2 else 'dve')
                        p.dma(ysc[e, sc * 128:(sc + 1) * 128, dh * 512:(dh + 1) * 512], o)
        p.flush()

    def phase_combine(self, l, si, xsrc, xdst):
        p = self.p
        Tn = T if si == 0 else TC
        NB = min(512, Tn)
        cap = 2 * Tn // NE
        Mp = cap if cap >= 128 else 128
        JC = Mp // 128
        sfx = '%d_%d' % (l, si)
        ysc = self.S('y' + sfx, [NE, Mp, D], BF16)
        posT = self.S('posT' + sfx, [NE, Tn])
        affTd = self.S('affT' + sfx, [NE, Tn])
        p.begin()
        g2 = self.load_modcols(l, si, 5, 'g2')
        jcol = p.tile([128, 4], F32, 'jcol')
        p.dma(jcol, self.jcol)
        yh = [p.tile([128, JC, 512], BF16, 'yh') for _ in range(NE)]
        posr = [p.tile([128, 8, NB], F32, 'posb') for _ in range(2)]
        affr = [p.tile([128, 8, NB], F32, 'affb') for _ in range(2)]
        xbr = Ring([p.tile([128, 4, NB], F32, 'xb') for _ in range(2)])
        sgr = Ring([p.tile([128, NB], BF16, 'sg') for _ in range(4)])
        o32 = Ring([p.tile([128, NB], F32, 'xo') for _ in range(4)])
        accs = Ring([[p.psum([128, 512], F32, 'cacc') for _ in range(4)] for _ in range(2)])
        yv = ysc.re("e (j q) d -> q e j d", q=128)
        ntb = Tn // NB
        steps = [(dh, tb) for dh in range(2) for tb in range(ntb)]

        def load_pa(step, eh):
            dh, tb = steps[step]
            t0 = tb * NB
            es = slice(eh * 8, (eh + 1) * 8)
            p.dma(posr[eh], V(posT.buf, posT.ap[es, t0:t0 + NB].partition_broadcast(128)))
            p.dma(affr[eh], V(affTd.buf, affTd.ap[es, t0:t0 + NB].partition_broadcast(128)))

        load_pa(0, 0)
        load_pa(0, 1)
        for st, (dh, tb) in enumerate(steps):
            t0 = tb * NB
            if tb == 0:
                for e in range(NE):
                    p.dma(yh[e], yv[:, e, :, dh * 512:(dh + 1) * 512])
            xb = xbr.next()
            p.dma(xb, xsrc[dh * 512:(dh + 1) * 512, t0:t0 + NB].re("(k q) t -> q k t", q=128))
            acc = accs.next()
            n = NE * JC
            idx = 0
            for eh in range(2):
                for e8 in range(8):
                    e = eh * 8 + e8
                    for jc in range(JC):
                        sg = sgr.next()
                        p.stt(sg, posr[eh][:, e8, :], jcol[:, jc:jc + 1], affr[eh][:, e8, :], ALU.is_equal,
                              ALU.mult)
                        for dc in range(4):
                            p.mm(acc[dc][:, 0:NB], yh[e][:, jc, dc * 128:(dc + 1) * 128], sg, idx == 0, idx == n - 1)
                        idx += 1
                if st + 1 < len(steps):
                    load_pa(st + 1, eh)
            for dc in range(4):
                c = dh * 4 + dc
                xo = o32.next()
                p.stt(xo, acc[dc][:, 0:NB], g2[:, c:c + 1], xb[:, dc, :], ALU.mult, ALU.add)
                p.dma(xdst[c * 128:(c + 1) * 128, t0:t0 + NB], xo)
        p.flush()

    def phase_final(self, xsrc):
        p = self.p
        p.begin()
        gs = p.tile([128, 8], F32, 'fg')
        self.col_load(gs, self.final_g)
        sh = p.tile([128, 8], F32, 'fsh')
        p.memset(sh, 0.0)
        ones = p.tile([128, 128], F32, 'ones')
        p.memset(ones, 1.0)
        self.epsc = p.tile([128, 1], F32, 'epsc')
        p.memset(self.epsc, EPS)
        NB = 512
        hring = Ring([p.tile([128, 8, NB], F32, 'oT') for _ in range(2)])
        xring = Ring([p.tile([128, 8, NB], F32, 'xb') for _ in range(2)])
        sqring = Ring([p.tile([128, NB], F32, 'sq') for _ in range(3)])
        tmpring = Ring([p.tile([128, NB], F32, 'tmp') for _ in range(3)])
        rsring = Ring([p.tile([128, NB], F32, 'rs') for _ in range(2)])
        psn = Ring([p.psum([128, 512], F32, 'psn') for _ in range(2)])
        xq = {0: self.norm_load(xsrc, 0, NB, xring)}
        for bi in range(T // NB):
            hT = hring.next()
            if bi + 1 < T // NB:
                xq[bi + 1] = self.norm_load(xsrc, (bi + 1) * NB, NB, xring)
            self.emit_norm(xsrc, bi * NB, NB, gs, sh, hT, 0, ones, xring, sqring, tmpring, psn, rsring, xb=xq[bi])
            p.dma(self.out[:, bi * NB:(bi + 1) * NB].re("(k q) t -> q k t", q=128), hT)
        p.flush()

    def build(self):
        self.phase_adaln()
        if self.stop == 'adaln':
            return
        x, cx = self.xT, self.ctxT
        for l in range(DEPTH):
            fc = l < DEPTH - 1
            st = lambda s: self.stop == '%s%d' % (s, l)
            self.phase_normproj(l, x, cx, fc)
            if st('proj'):
                return
            self.phase_attn(l, fc)
            if st('attn'):
                return
            self.phase_fourier_pool(l, 0)
            if fc:
                self.phase_fourier_pool(l, 1)
            if st('four'):
                return
            xm = self.S('xm%d' % l, [D, T])
            cm = self.S('cm%d' % l, [D, TC])
            self.phase_merge(l, 0, x, xm)
            if fc:
                self.phase_merge(l, 1, cx, cm)
            if st('merge'):
                return
            self.phase_route(l, 0, xm)
            if fc:
                self.phase_route(l, 1, cm)
            if st('route'):
                return
            streams = [0, 1] if fc else [0]
            self.phase_gather(l, streams)
            if st('gather'):
                return
            self.phase_experts(l, streams)
            if st('experts'):
                return
            xe = self.S('xe%d' % l, [D, T])
            ce = self.S('ce%d' % l, [D, TC])
            self.phase_combine(l, 0, xm, xe)
            if fc:
                self.phase_combine(l, 1, cm, ce)
            if st('combine'):
                return
            x, cx = xe, ce
        self.phase_final(x)


def build_nc(dbg=(), stop=None):
    nc = bass.Bass("TRN2", target_bir_lowering=False)
    nc.dge_precook = False
    stack = ExitStack()
    with stack:
        net = Net(nc, stack, dbg=dbg, stop=stop)
        net.build()
    return nc, net


def chunk_tiles(j):
    if j == 0:
        return list(range(0, 6)), 0
    if j == 7:
        return list(range(26, 32)), 14
    return list(range(4 * j - 2, 4 * j + 6)), 6


_CONSTS = None


def make_consts():
    global _CONSTS
    if _CONSTS is not None:
        return _CONSTS
    import ml_dtypes
    out = {}
    for name, L in (('', T), ('_c', TC)):
        idx = (np.arange(L, dtype=np.int64)[:, None] * np.arange(L, dtype=np.int64)[None, :]) % L
        base = 2.0 * np.pi * np.arange(L, dtype=np.float64) / L
        out['dftc' + name] = (np.cos(base)[idx] / np.sqrt(L)).astype(np.float32).astype(ml_dtypes.bfloat16)
        out['dfts' + name] = (np.sin(base)[idx] / np.sqrt(L)).astype(np.float32).astype(ml_dtypes.bfloat16)
        t = np.arange(L)
        ic = np.zeros((4, L), np.float32)
        for g, w in enumerate((2, 4, 8, 16)):
            lo = np.clip(t - w // 2, 0, L)
            hi = np.clip(t + w - w // 2, 0, L)
            ic[g] = 1.0 / (hi - lo)
        out['invcnt' + name] = ic
    ang = 2.0 * np.pi * ((np.arange(128)[:, None] * np.arange(128)[None, :]) % 128) / 128.0
    out['dft128'] = (np.concatenate([np.cos(ang), np.sin(ang)], axis=1) / np.sqrt(128.0)).astype(np.float32)
    out['ident'] = np.eye(128, dtype=np.float32)
    out['iota512'] = np.ascontiguousarray(np.broadcast_to(np.arange(512, dtype=np.float32), (128, 512)))
    out['jcol'] = (np.arange(4, dtype=np.float32)[None, :] * 128 + np.arange(128, dtype=np.float32)[:, None])
    _CONSTS = out
    return out


def make_biasx(rpb):
    a = np.arange(2)[:, None, None, None]
    kc = np.arange(64)[None, :, None, None]
    rl = np.arange(8)[None, None, :, None]
    c = np.arange(64)[None, None, None, :]
    cs = np.clip(c - 8, 0, 48)
    colok = (kc >= cs) & (kc < cs + 16)
    coff = np.clip(kc - c + 15, 0, 30)
    out = np.full((DEPTH, NH, 20, 2, 64, 8, 64), NEG, np.float32)
    for j in (0, 1, 7):
        tiles, base = chunk_tiles(j)
        for ti, m in enumerate(tiles):
            kr = 2 * m + a
            r = 8 * j + rl
            rs = np.clip(r - 4, 0, 56)
            rowok = (kr >= rs) & (kr <= rs + 7)
            roff = np.clip(kr - r + 7, 0, 14)
            ok = np.broadcast_to(rowok & colok, (2, 64, 8, 64))
            ro = np.broadcast_to(roff, (2, 64, 8, 64))
            co = np.broadcast_to(coff, (2, 64, 8, 64))
            vals = rpb[:, :, ro, co]
            out[:, :, base + ti] = np.where(ok[None, None], vals, np.float32(NEG))
    return np.ascontiguousarray(out.reshape(DEPTH * NH, 20, 128, 512))


def host_shared(inputs):
    sh = dict(make_consts())
    f = lambda k: np.ascontiguousarray(np.asarray(inputs[k], np.float32))
    sh['c_ctx'] = f('c_ctx').reshape(1, D)
    for k in ('ada_w', 'ada_b', 'norm1_g', 'norm2_g', 'w_in', 'w_att_o', 'w_fourier', 'w_pool', 'pool_scale',
              'w_out', 'w_router'):
        sh[k] = f(k)
    sh['w_g'] = f('w_exp_gate').reshape(DEPTH * NE, D, FF)
    sh['w_u'] = f('w_exp_up').reshape(DEPTH * NE, D, FF)
    sh['w_d'] = f('w_exp_down').reshape(DEPTH * NE, FF, D)
    sh['final_g'] = f('final_norm_g').reshape(1, D)
    sh['biasx'] = make_biasx(f('rpb'))
    return sh


def core_inputs(inputs, b, shared, names):
    m = {}
    for n in names:
        if n == 'xT':
            m[n] = np.ascontiguousarray(np.asarray(inputs['x'][b], np.float32).T)
        elif n == 'ctxT':
            m[n] = np.ascontiguousarray(np.asarray(inputs['ctx'][b], np.float32).T)
        elif n == 'c':
            m[n] = np.ascontiguousarray(np.asarray(inputs['c'][b], np.float32)).reshape(1, D)
        else:
            m[n] = shared[n]
    return m


def kernel(**inputs):
    nc, net = build_nc()
    shared = host_shared(inputs)
    names = list(net.ins.keys())
    in_maps = [core_inputs(inputs, b, shared, names) for b in range(8)]
    res = run_bass_kernel_spmd(nc, in_maps, core_ids=list(range(8)))
    out = np.stack([np.ascontiguousarray(r['outT'].T) for r in res.results], axis=0)
    return out.astype(np.float32)
```
